# Optimizing a Trainium2 kernel written in Bass

```python
import jax, jax.numpy as jnp
from jax import lax
import numpy as np

D_MODEL = 1024
BATCH = 32
SEQ = 2048
DEPTH = 4

ATT_HEADS = 8
HEAD_DIM = 64
ATT_WIDTH = ATT_HEADS * HEAD_DIM
IDX_HEADS = 8
IDX_DIM = 64
TOPK_MAX = 256
Q_BLOCK = 128
CONV_WIDTH = D_MODEL // 2
CONV_KERNEL = 31
SHORT_WIDTH = D_MODEL
SHORT_KERNEL = 3
ROPE_THETA = 10000.0
EPS = 1e-6
EVEN_SPLIT = (ATT_WIDTH, HEAD_DIM, HEAD_DIM, IDX_HEADS * IDX_DIM, IDX_DIM, IDX_HEADS,
              ATT_WIDTH, 2 * CONV_WIDTH, CONV_WIDTH)
EVEN_IN = sum(EVEN_SPLIT)
EVEN_MIX = ATT_WIDTH + CONV_WIDTH
ODD_IN = 4 * SHORT_WIDTH
N_EVEN = (DEPTH + 1) // 2
N_ODD = DEPTH // 2

kernel_name = "hybrid_dsa_conformer_shortconv_trunk"


def _split(u, sizes):
    idx = np.cumsum(np.array(sizes))[:-1].tolist()
    return jnp.split(u, idx, axis=-1)


def rms_norm(x, g):
    xf = x.astype(jnp.float32)
    y = xf * lax.rsqrt(jnp.mean(xf * xf, axis=-1, keepdims=True) + EPS)
    return (y * g.astype(jnp.float32)).astype(x.dtype)


def layer_norm(x, g, b):
    xf = x.astype(jnp.float32)
    mu = jnp.mean(xf, axis=-1, keepdims=True)
    xc = xf - mu
    var = jnp.mean(xc * xc, axis=-1, keepdims=True)
    y = xc * lax.rsqrt(var + EPS) * g.astype(jnp.float32) + b.astype(jnp.float32)
    return y.astype(x.dtype)


def rope(x, pos):
    d = x.shape[-1]
    half = d // 2
    inv = ROPE_THETA ** (-jnp.arange(half, dtype=jnp.float32) / half)
    ang = pos.astype(jnp.float32)[..., None] * inv
    ang = ang.reshape(ang.shape[:2] + (1,) * (x.ndim - 3) + (half,))
    cos, sin = jnp.cos(ang), jnp.sin(ang)
    xf = x.astype(jnp.float32)
    x1, x2 = xf[..., :half], xf[..., half:]
    out = jnp.concatenate([x1 * cos - x2 * sin, x2 * cos + x1 * sin], axis=-1)
    return out.astype(x.dtype)


def causal_depthwise_conv(x, w):
    K, C = w.shape
    return lax.conv_general_dilated(
        x, w[:, None, :].astype(x.dtype), window_strides=(1,), padding=((K - 1, 0),),
        dimension_numbers=('NWC', 'WIO', 'NWC'), feature_group_count=C)


def dsa_attention(q, k, v, q_idx, k_idx, w_idx):
    Bn, S, H, dh = q.shape
    L = S
    topk = min(TOPK_MAX, L // 4)
    nb = S // Q_BLOCK
    kf, vf, kif = k.astype(jnp.float32), v.astype(jnp.float32), k_idx.astype(jnp.float32)
    key_pos = jnp.arange(L)

    def to_blocks(a):
        return a.reshape((Bn, nb, Q_BLOCK) + a.shape[2:]).swapaxes(0, 1)

    def block(args):
        qb, qib, wb, t = args
        dots = jnp.einsum('bqhd,bsd->bqhs', qib.astype(jnp.float32), kif)
        score = jnp.einsum('bqh,bqhs->bqs', wb.astype(jnp.float32), jax.nn.relu(dots))
        causal = key_pos[None, :] <= t[:, None]
        score = jnp.where(causal[None], score, -jnp.inf)
        _, sel = lax.top_k(score, topk)
        kg = jax.vmap(lambda kb, ib: kb[ib])(kf, sel)
        vg = jax.vmap(lambda vb, ib: vb[ib])(vf, sel)
        logits = jnp.einsum('bqhd,bqkd->bqhk', qb.astype(jnp.float32), kg) * (dh ** -0.5)
        valid = sel <= t[None, :, None]
        logits = jnp.where(valid[:, :, None, :], logits, -jnp.inf)
        p = jax.nn.softmax(logits, axis=-1)
        return jnp.einsum('bqhk,bqkd->bqhd', p, vg).astype(q.dtype)

    t_blocks = jnp.arange(S).reshape(nb, Q_BLOCK)
    out = lax.map(block, (to_blocks(q), to_blocks(q_idx), to_blocks(w_idx), t_blocks))
    return out.swapaxes(0, 1).reshape(Bn, S, H, dh)


def setup_inputs(seed: int = 0) -> dict:
    key = jax.random.key(seed)
    ks = jax.random.split(key, 16)

    def nrm(k, shape, s):
        return jax.random.normal(k, shape, jnp.float32) * s

    x = jax.random.normal(ks[0], (BATCH, SEQ, D_MODEL), jnp.float32)
    positions = jnp.tile(jnp.arange(SEQ, dtype=jnp.int32)[None, :], (BATCH, 1))
    norm_g = 1.0 + nrm(ks[1], (DEPTH, D_MODEL), 0.02)
    w_in_even = nrm(ks[2], (N_EVEN, D_MODEL, EVEN_IN), D_MODEL ** -0.5)
    w_out_even = nrm(ks[3], (N_EVEN, EVEN_MIX, D_MODEL), EVEN_MIX ** -0.5)
    conv_b_w = nrm(ks[4], (N_EVEN, CONV_KERNEL, CONV_WIDTH), CONV_KERNEL ** -0.5)
    conv_b_bias = nrm(ks[5], (N_EVEN, CONV_WIDTH), 0.02)
    conv_ln_g = 1.0 + nrm(ks[6], (N_EVEN, CONV_WIDTH), 0.02)
    conv_ln_b = nrm(ks[7], (N_EVEN, CONV_WIDTH), 0.02)
    w_in_odd = nrm(ks[8], (N_ODD, D_MODEL, ODD_IN), D_MODEL ** -0.5)
    conv_c_w = nrm(ks[9], (N_ODD, SHORT_KERNEL, SHORT_WIDTH), SHORT_KERNEL ** -0.5)
    w_out_odd = nrm(ks[10], (N_ODD, SHORT_WIDTH, D_MODEL), SHORT_WIDTH ** -0.5)
    final_g = 1.0 + nrm(ks[11], (D_MODEL,), 0.02)
    return {"x": x, "positions": positions, "norm_g": norm_g,
            "w_in_even": w_in_even, "w_out_even": w_out_even,
            "conv_b_w": conv_b_w, "conv_b_bias": conv_b_bias,
            "conv_ln_g": conv_ln_g, "conv_ln_b": conv_ln_b,
            "w_in_odd": w_in_odd, "conv_c_w": conv_c_w, "w_out_odd": w_out_odd,
            "final_g": final_g}


def even_layer(h, pos, w_in, w_out, cb_w, cb_bias, ln_g, ln_b):
    Bn, S, _ = h.shape
    u = h @ w_in
    q, k, v, qi, ki, wi, gate_a, glu_b, gate_b = _split(u, EVEN_SPLIT)
    q = rope(q.reshape(Bn, S, ATT_HEADS, HEAD_DIM), pos)
    k = rope(k, pos)
    qi = rope(qi.reshape(Bn, S, IDX_HEADS, IDX_DIM), pos)
    ki = rope(ki, pos)
    wi = wi * (IDX_HEADS ** -0.5 * IDX_DIM ** -0.5)
    a = dsa_attention(q, k, v, qi, ki, wi).reshape(Bn, S, ATT_WIDTH)
    a = a * jax.nn.silu(gate_a)
    g_lin, g_gate = jnp.split(glu_b, 2, axis=-1)
    c = g_lin * jax.nn.sigmoid(g_gate)
    c = causal_depthwise_conv(c, cb_w) + cb_bias
    c = jax.nn.silu(layer_norm(c, ln_g, ln_b))
    c = c * jax.nn.silu(gate_b)
    return jnp.concatenate([a, c], axis=-1) @ w_out


def odd_layer(h, w_in, conv_w, w_out):
    u = h @ w_in
    b_gate, c_gate, xin, gate = jnp.split(u, 4, axis=-1)
    y = b_gate * causal_depthwise_conv(c_gate * xin, conv_w)
    return (y * jax.nn.silu(gate)) @ w_out


def reference(x, positions, norm_g, w_in_even, w_out_even, conv_b_w, conv_b_bias,
              conv_ln_g, conv_ln_b, w_in_odd, conv_c_w, w_out_odd, final_g):
    for l in range(DEPTH):
        h = rms_norm(x, norm_g[l])
        i = l // 2
        if l % 2 == 0:
            x = x + even_layer(h, positions, w_in_even[i], w_out_even[i], conv_b_w[i],
                               conv_b_bias[i], conv_ln_g[i], conv_ln_b[i])
        else:
            x = x + odd_layer(h, w_in_odd[i], conv_c_w[i], w_out_odd[i])
    return rms_norm(x, final_g)
```

```python
import numpy as np
import concourse.bass as bass
import concourse.mybir as mybir
from concourse.bass_utils import run_bass_kernel_spmd
from contextlib import ExitStack

dt = mybir.dt
F32, BF16, I32 = dt.float32, dt.bfloat16, dt.int32
AF = mybir.ActivationFunctionType
ALU = mybir.AluOpType
AX = mybir.AxisListType

import os as _os
SEM_LIMIT = int(_os.environ.get("MK_SEMLIM", 1000000))


class Res:
    __slots__ = ("name", "w", "r", "excl")

    def __init__(self, name, excl=False):
        self.name = name
        self.w = None
        self.r = {}
        self.excl = excl


class _Eng:
    def __init__(self, name, h, sems):
        self.name = name
        self.h = h
        self.sems = sems
        self.si = 0
        self.cnt = 0
        self.seen = {}
        self.last = None


class Sched:
    def __init__(self, nc, stack, n_eng_sems=14, n_dma_sems=24):
        self.nc = nc
        self.engs = {}
        hs = {"pe": nc.tensor, "act": nc.scalar, "dve": nc.vector, "pool": nc.gpsimd, "sp": nc.sync}
        for n, h in hs.items():
            sems = [stack.enter_context(nc.semaphore(f"s_{n}_{i}")) for i in range(n_eng_sems if n != "sp" else 1)]
            self.engs[n] = _Eng(n, h, sems)
        self.dsems = [stack.enter_context(nc.semaphore(f"s_dma_{i}")) for i in range(n_dma_sems)]
        self.dma_i = 0
        self.dma_tokens = []
        self.n_wait = 0
        self.n_ins = 0

    def res(self, name):
        return Res(name)

    def _wait(self, E, tok):
        sem, val, en = tok
        if E.seen.get(id(sem), 0) >= val:
            return
        E.h.wait_ge(sem, val)
        E.seen[id(sem)] = val
        self.n_wait += 1

    def _deps(self, E, en, reads, writes):
        for r in reads:
            if r.w is not None:
                self._wait(E, r.w)
            if r.excl:
                for k, t in r.r.items():
                    if k != en:
                        self._wait(E, t)
        for w in writes:
            if w.w is not None and not (en == "pe" and w.w[2] == "pe"):
                self._wait(E, w.w)
            for t in w.r.values():
                self._wait(E, t)

    def _record(self, tok, key, reads, writes):
        for r in reads:
            r.r[key] = tok
        for w in writes:
            w.w = tok
            w.r = {}

    def op(self, en, fn, reads=(), writes=()):
        E = self.engs[en]
        self._deps(E, en, reads, writes)
        ins = fn(E.h)
        if E.cnt >= SEM_LIMIT:
            E.si += 1
            E.cnt = 0
        sem = E.sems[E.si]
        E.cnt += 1
        ins.then_inc(sem, 1)
        tok = (sem, E.cnt, en)
        E.last = tok
        self.n_ins += 1
        self._record(tok, en, reads, writes)
        return tok

    def dma(self, qn, out, in_, reads=(), writes=()):
        E = self.engs[qn]
        self._deps(E, qn, reads, writes)
        i = self.dma_i
        self.dma_i += 1
        nd = len(self.dsems)
        sem = self.dsems[i % nd]
        val = 16 * (i // nd + 1)
        if i >= nd:
            self._wait(E, (sem, val - 16, "dma"))
        ins = E.h.dma_start(out=out, in_=in_)
        ins.then_inc(sem, 16)
        tok = (sem, val, "dma")
        self.dma_tokens.append(tok)
        self.n_ins += 1
        self._record(tok, ("dma", i), reads, writes)
        return tok

    def finish(self):
        E = self.engs["sp"]
        for tok in self.dma_tokens[-len(self.dsems):]:
            self._wait(E, tok)
        for n, e in self.engs.items():
            if e.last is not None:
                self._wait(E, e.last)


D = 1024
S_LEN = 2048
TB = 512
NBLK = S_LEN // TB
NCH_E = 35
NCH_O = 40
LBASE = [0, 35, 75, 110]
NCH = 150
NW = 6
NIT = 16
TOPK = 256
P_G0, P_GF, P_CBW, P_CBB, P_LNG, P_LNB, P_CCW, P_INV, P_EPS, P_CK = 0, 32, 40, 288, 296, 304, 312, 360, 361, 368
NPAR = 400
TWO_PI = 6.283185307179586
PI = 3.141592653589793


class _Tile:
    __slots__ = ("t", "res")

    def __init__(self, t, res):
        self.t = t
        self.res = res


class _Rot:
    def __init__(self, tiles):
        self.tiles = tiles
        self.i = 0

    def next(self):
        t = self.tiles[self.i % len(self.tiles)]
        self.i += 1
        return t


def build_program(NS, NL):
    nc = bass.Bass("TRN2", target_bir_lowering=False)
    x_d = nc.dram_tensor("x", [NS, S_LEN, D], F32, kind="ExternalInput").ap()
    pos_d = nc.dram_tensor("pos", [NS, S_LEN], I32, kind="ExternalInput").ap()
    wall_d = nc.dram_tensor("wall", [NCH, 128, 1024], F32, kind="ExternalInput").ap()
    par_d = nc.dram_tensor("par", [128, NPAR], F32, kind="ExternalInput").ap()
    cst_d = nc.dram_tensor("cst", [128, 384], F32, kind="ExternalInput").ap()
    out_d = nc.dram_tensor("out", [NS, S_LEN, D], F32, kind="ExternalOutput").ap()
    wbf_d = nc.dram_tensor("wbf", [NCH, 128, 1024], BF16, kind="Internal").ap()
    dgd_d = nc.dram_tensor("dgd", [8, 128, 31 * 128], BF16, kind="Internal").ap()

    with ExitStack() as st:
        S = Sched(nc, st)
        R = S.res

        def sb(name, shape, dtype):
            return nc.alloc_sbuf_tensor("sb_" + name, shape, dtype)

        def mk(name, shape, dtype):
            return _Tile(sb(name, shape, dtype), R(name))

        def mkrot(name, n, shape, dtype):
            return _Rot([mk(f"{name}{i}", shape, dtype) for i in range(n)])

        par = mk("par", [128, NPAR], F32)
        cst = mk("cst", [128, 384], F32)
        identF = cst.t[:, 0:128]
        causneg = cst.t[:, 256:384]
        cb = mk("cb", [128, 4, 128], BF16)
        identB, onesB, bigI, RmB = cb.t[:, 0, :], cb.t[:, 1, :], cb.t[:, 2, :], cb.t[:, 3, :]
        xT = sb("xT", [128, 8, TB], F32)
        xT_r = [R(f"xT{k}") for k in range(8)]
        hT = sb("hT", [128, 8, TB], BF16)
        hT_r = [R(f"hT{k}") for k in range(8)]
        yT = sb("yT", [128, 8, TB], BF16)
        yT_r = [R(f"yT{k}") for k in range(8)]
        rstd = mk("rstd", [128, TB], F32)
        cosT = mk("cosT", [128, TB], F32)
        sinT = mk("sinT", [128, TB], F32)
        kdup = [sb(f"kdup{i}", [128, S_LEN], BF16) for i in range(2)]
        kidup = [sb(f"kidup{i}", [128, S_LEN], BF16) for i in range(2)]
        kdup_r = [[R(f"kdup{i}_{b}") for b in range(NBLK)] for i in range(2)]
        kidup_r = [[R(f"kidup{i}_{b}") for b in range(NBLK)] for i in range(2)]
        vaug = [sb(f"vaug{i}", [128, 16, 65], BF16) for i in range(2)]
        vaug_r = [[R(f"vaug{i}_{b}") for b in range(NBLK)] for i in range(2)]
        qz = mk("qz", [128, 8, TB], BF16)
        qiz = mk("qiz", [128, 8, TB], BF16)
        qz_r = [R(f"qz{h}") for h in range(8)]
        qiz_r = [R(f"qiz{h}") for h in range(8)]
        sgA = sb("sgA", [128, 4, TB], BF16)
        sgA_r = [R(f"sgA{c}") for c in range(4)]
        sgB = sb("sgB", [128, 4, TB], BF16)
        sgB_r = [R(f"sgB{c}") for c in range(4)]
        cglu = sb("cglu", [128, 4, 30 + TB], BF16)
        cglu_r = [R(f"cglu{c}") for c in range(4)]
        chal = mk("chal", [128, 2, 4, 30], BF16)
        zhal = mk("zhal", [128, 2, 8, 2], BF16)
        xc = sb("xc", [128, 4, TB], BF16)
        xc_r = [R(f"xc{c}") for c in range(4)]
        witok = mk("witok", [128, 4, 8], F32)
        wring = mkrot("wr", NW, [128, 8, 128], BF16)
        DG = mkrot("dg", 2, [128, 31, 128], BF16)
        DG3 = mkrot("dg3", 2, [128, 3, 128], BF16)
        DW = mkrot("dgw", 2, [128, 8, 128], BF16)
        ZB = mkrot("zb", 2, [128, 2 + TB], BF16)
        ACC = mkrot("acc", 2, [128, S_LEN], F32)
        NM = mkrot("nm", 4, [128, S_LEN], BF16)
        R16 = mkrot("r16", 3, [128, 512], BF16)
        PT = mkrot("pt", 3, [128, 512], BF16)
        T32 = mkrot("t32", 8, [128, 512], F32)
        T16 = mkrot("t16", 4, [128, 512], BF16)
        XIN = mkrot("xin", 2, [128, 1024], F32)
        WST = mkrot("wst", 2, [128, 1024], BF16)
        atok = mk("atok", [128, 8, 64], BF16)
        SM = mkrot("sm", 2, [128, 32], F32)
        rinv = mk("rinv", [128, 8], F32)
        posi = mk("posi", [128, TB], I32)
        RT = T32
        RTI = mk("rti", [128, TB], I32)

        banks = [_Tile(nc.alloc_psum_tensor(f"ps{i}", [128, 512], F32), Res(f"ps{i}", excl=True)) for i in range(8)]
        PS = _Rot(banks)
        PA = _Rot(banks[0:2])
        PD = _Rot(banks[2:4])
        PO = banks[4:6]
        PL = _Rot(banks[6:8])

        wbf_r = [R(f"wbf{c}") for c in range(NCH)]
        dgd_r = [R(f"dgd{c}") for c in range(8)]

        def op(en, fn, reads=(), writes=()):
            return S.op(en, fn, list(reads), list(writes))

        S.dma("sp", par.t[:, :], par_d, [], [par.res])
        S.dma("sp", cst.t[:, :], cst_d, [], [cst.res])
        n_cast = LBASE[NL] if NL < 4 else NCH
        cast_engs = ["act", "dve", "pool"]
        pp = {"next": 0}

        def prepass_one():
            c = pp["next"]
            if c >= n_cast:
                return
            pp["next"] += 1
            sf = XIN.next()
            sbf = WST.next()
            S.dma("sp", sf.t[:, :], wall_d[c], [], [sf.res])
            en = cast_engs[c % 3]
            if en == "act":
                op("act", lambda e: e.activation(out=sbf.t[:, :], in_=sf.t[:, :], func=AF.Copy), [sf.res], [sbf.res])
            else:
                op(en, lambda e: e.tensor_copy(out=sbf.t[:, :], in_=sf.t[:, :]), [sf.res], [sbf.res])
            S.dma("sp", wbf_d[c], sbf.t[:, :], [sbf.res], [wbf_r[c]])

        op("dve", lambda e: e.tensor_copy(out=identB, in_=identF), [cst.res], [cb.res])
        op("dve", lambda e: e.memset(onesB, 1.0), [], [cb.res])
        op("dve", lambda e: e.tensor_scalar(out=bigI, in0=identF, scalar1=32768.0, scalar2=None, op0=ALU.mult), [cst.res], [cb.res])
        op("dve", lambda e: e.tensor_copy(out=RmB, in_=cst.t[:, 128:256]), [cst.res], [cb.res])
        op("dve", lambda e: e.memset(qz.t[:, :, :], 0.0), [], [qz.res] + qz_r)
        op("dve", lambda e: e.memset(qiz.t[:, :, :], 0.0), [], [qiz.res] + qiz_r)
        for i in range(2):
            op("dve", lambda e: e.memset(vaug[i][:, :, :], 1.0), [], vaug_r[i])

        for i8 in range(8):
            dg = DG.next()
            for j in range(31):
                wcol = P_CBW + i8 * 31 + j
                op("pool", lambda e: e.tensor_scalar(out=dg.t[:, j, :], in0=identB, scalar1=par.t[:, wcol:wcol + 1], scalar2=0.0,
                                                     op0=ALU.mult, op1=ALU.add), [cb.res, par.res], [dg.res])
            S.dma("sp", dgd_d[i8], dg.t[:, :, :].rearrange("p a b -> p (a b)"), [dg.res], [dgd_r[i8]])

        import os
        STG = float(os.environ.get("MK_STAGE", 99))
        per_blk = LBASE[NL] if NL < 4 else NCH
        if STG < 6:
            per_blk = {1.0: 12, 1.2: 16, 1.4: 17, 1.6: 18}.get(STG, 27)
        total_w = NS * NBLK * per_blk
        ws = {"load": 0, "use": 0}

        def w_load_more():
            while ws["load"] < total_w and ws["load"] < ws["use"] + NW:
                i = ws["load"]
                ch = i % per_blk
                assert wbf_r[ch].w is not None, ch
                slot = wring.tiles[i % NW]
                S.dma("sp", slot.t[:, :, :], wbf_d[ch].rearrange("p (k m) -> p k m", k=8), [wbf_r[ch]], [slot.res])
                ws["load"] += 1

        def w_use(ch):
            i = ws["use"]
            assert i % per_blk == ch, (i, per_blk, ch)
            ws["use"] += 1
            return wring.tiles[i % NW]

        def proj(chs, rhs, rhs_r):
            outs = []
            for ch in chs:
                w = w_use(ch)
                ps = PS.next()
                for kc in range(8):
                    op("pe", lambda e: e.matmul(ps.t[:, :], w.t[:, kc, :], rhs[:, kc, :], start=(kc == 0), stop=(kc == 7)),
                       [w.res, rhs_r[kc]], [ps.res])
                w_load_more()
                prepass_one()
                outs.append(ps)
            return outs

        def load_x(s, tok0):
            for tt in range(4):
                xin = XIN.next()
                S.dma("sp", xin.t[:, :], x_d[s, tok0 + tt * 128: tok0 + (tt + 1) * 128, :], [], [xin.res])
                for half in range(2):
                    ps = PS.next()
                    for j in range(4):
                        kc = half * 4 + j
                        op("pe", lambda e: e.transpose(ps.t[:, j * 128:(j + 1) * 128], xin.t[:, kc * 128:(kc + 1) * 128], identF),
                           [xin.res, cst.res], [ps.res])
                    op("act", lambda e: e.activation(out=xT[:, half * 4:half * 4 + 4, tt * 128:(tt + 1) * 128],
                                                     in_=ps.t[:, :].rearrange("p (a b) -> p a b", a=4), func=AF.Copy),
                       [ps.res], xT_r[half * 4:half * 4 + 4])

        def rope_tables(s, tok0):
            S.dma("sp", posi.t[:, :], pos_d[s:s + 1, tok0:tok0 + TB].partition_broadcast(128), [], [posi.res])
            posf = RT.next()
            op("pool", lambda e: e.tensor_copy(out=posf.t[:, :], in_=posi.t[:, :]), [posi.res], [posf.res])
            ang = RT.next()
            op("pool", lambda e: e.tensor_scalar(out=ang.t[:, :], in0=posf.t[:, :], scalar1=par.t[:, P_INV:P_INV + 1], scalar2=0.0,
                                                 op0=ALU.mult, op1=ALU.add), [posf.res, par.res], [ang.res])
            for dst, shift in ((sinT, 0.0), (cosT, PI / 2)):
                t = RT.next()
                op("pool", lambda e: e.tensor_scalar(out=t.t[:, :], in0=ang.t[:, :], scalar1=1.0 / TWO_PI, scalar2=shift / TWO_PI,
                                                     op0=ALU.mult, op1=ALU.add), [ang.res], [t.res])
                op("pool", lambda e: e.tensor_copy(out=RTI.t[:, :], in_=t.t[:, :]), [t.res], [RTI.res])
                op("pool", lambda e: e.tensor_copy(out=t.t[:, :], in_=RTI.t[:, :]), [RTI.res], [t.res])
                r = RT.next()
                op("dve", lambda e: e.scalar_tensor_tensor(out=r.t[:, :], in0=t.t[:, :], scalar=-TWO_PI, in1=ang.t[:, :],
                                                           op0=ALU.mult, op1=ALU.add), [t.res, ang.res], [r.res])
                if shift != 0.0:
                    op("dve", lambda e: e.tensor_scalar(out=r.t[:, :], in0=r.t[:, :], scalar1=shift, scalar2=None, op0=ALU.add),
                       [r.res], [r.res])
                op("dve", lambda e: e.tensor_scalar(out=t.t[:, :], in0=r.t[:, :], scalar1=PI, scalar2=-TWO_PI, op0=ALU.is_gt, op1=ALU.mult),
                   [r.res], [t.res])
                op("dve", lambda e: e.tensor_tensor(out=r.t[:, :], in0=r.t[:, :], in1=t.t[:, :], op=ALU.add), [r.res, t.res], [r.res])
                op("dve", lambda e: e.tensor_scalar(out=t.t[:, :], in0=r.t[:, :], scalar1=-PI, scalar2=TWO_PI, op0=ALU.is_lt, op1=ALU.mult),
                   [r.res], [t.res])
                op("dve", lambda e: e.tensor_tensor(out=r.t[:, :], in0=r.t[:, :], in1=t.t[:, :], op=ALU.add), [r.res, t.res], [r.res])
                op("dve", lambda e: e.tensor_scalar(out=r.t[:, :], in0=r.t[:, :], scalar1=3.1415925, scalar2=-3.1415925, op0=ALU.min, op1=ALU.max),
                   [r.res], [r.res])
                op("act", lambda e: e.activation(out=dst.t[:, :], in_=r.t[:, :], func=AF.Sin), [r.res], [dst.res])

        def norm_stats():
            ps = PS.next()
            for kc in range(8):
                sq = T16.next()
                op("act", lambda e: e.activation(out=sq.t[:, :], in_=xT[:, kc, :], func=AF.Square), [xT_r[kc]], [sq.res])
                op("pe", lambda e: e.matmul(ps.t[:, :], onesB, sq.t[:, :], start=(kc == 0), stop=(kc == 7)), [sq.res, cb.res], [ps.res])
            sd = T32.next()
            op("act", lambda e: e.activation(out=sd.t[:, :], in_=ps.t[:, :], func=AF.Sqrt, scale=1.0 / D, bias=par.t[:, P_EPS:P_EPS + 1]),
               [ps.res, par.res], [sd.res])
            op("dve", lambda e: e.reciprocal(out=rstd.t[:, :], in_=sd.t[:, :]), [sd.res], [rstd.res])

        def norm_to_hT(l):
            norm_stats()
            for kc in range(8):
                g = par.t[:, P_G0 + l * 8 + kc: P_G0 + l * 8 + kc + 1]
                op("dve", lambda e: e.scalar_tensor_tensor(out=hT[:, kc, :], in0=xT[:, kc, :], scalar=g, in1=rstd.t[:, :],
                                                           op0=ALU.mult, op1=ALU.mult), [xT_r[kc], rstd.res, par.res], [hT_r[kc]])

        def rope(ps, dests):
            u1 = T16.next()
            op("dve", lambda e: e.tensor_tensor(out=u1.t[:, :], in0=ps.t[:, :], in1=cosT.t[:, :], op=ALU.mult), [ps.res, cosT.res], [u1.res])
            u2 = T16.next()
            op("dve", lambda e: e.tensor_tensor(out=u2.t[:, :], in0=ps.t[:, :], in1=sinT.t[:, :], op=ALU.mult), [ps.res, sinT.res], [u2.res])
            ps2 = PS.next()
            op("pe", lambda e: e.matmul(ps2.t[:, :], identB, u1.t[:, :], start=True, stop=False), [u1.res, cb.res], [ps2.res])
            op("pe", lambda e: e.matmul(ps2.t[:, :], RmB, u2.t[:, :], start=False, stop=True), [u2.res, cb.res], [ps2.res])
            for rows, out_ap, res in dests:
                op("act", lambda e: e.activation(out=out_ap, in_=ps2.t[rows, :], func=AF.Copy), [ps2.res], [res])

        def out_proj(base):
            for m in range(8):
                (ps,) = proj([base + m], yT, yT_r)
                op("dve", lambda e: e.tensor_tensor(out=xT[:, m, :], in0=ps.t[:, :], in1=xT[:, m, :], op=ALU.add), [ps.res, xT_r[m]], [xT_r[m]])

        def even_layer(l, blk):
            li = l // 2
            base = LBASE[l]
            gi0 = blk * 4
            tok0 = blk * TB
            norm_to_hT(l)
            dgs = {}

            def dg_load(cc):
                dg = DG.next()
                S.dma("sp", dg.t[:, :, :].rearrange("p a b -> p (a b)"), dgd_d[li * 4 + cc], [dgd_r[li * 4 + cc]], [dg.res])
                dgs[cc] = dg
            dg_load(0)
            dg_load(1)
            if blk == 0:
                op("pool", lambda e: e.memset(cglu[:, :, 0:30], 0.0), [], cglu_r)
            else:
                op("pool", lambda e: e.tensor_copy(out=cglu[:, :, 0:30], in_=chal.t[:, li, :, :]), [chal.res], cglu_r)
            for c in range(4):
                ps_l, ps_g = proj([base + 2 * c, base + 2 * c + 1], hT, hT_r)
                sig = T32.next()
                op("act", lambda e: e.activation(out=sig.t[:, :], in_=ps_g.t[:, :], func=AF.Sigmoid), [ps_g.res], [sig.res])
                op("dve", lambda e: e.tensor_tensor(out=cglu[:, c, 30:30 + TB], in0=ps_l.t[:, :], in1=sig.t[:, :], op=ALU.mult),
                   [ps_l.res, sig.res], [cglu_r[c]])
            op("pool", lambda e: e.tensor_copy(out=chal.t[:, li, :, :], in_=cglu[:, :, TB:TB + 30]), cglu_r, [chal.res])
            for c in range(4):
                (ps,) = proj([base + 8 + c], hT, hT_r)
                op("act", lambda e: e.activation(out=sgB[:, c, :], in_=ps.t[:, :], func=AF.Silu), [ps.res], [sgB_r[c]])
            if STG <= 1:
                return
            for c in range(4):
                (ps,) = proj([base + 12 + c], hT, hT_r)

                rope(ps, [(slice(0, 64), qz.t[0:64, 2 * c, :], qz_r[2 * c]), (slice(64, 128), qz.t[64:128, 2 * c + 1, :], qz_r[2 * c + 1])])
            if STG <= 1.2:
                return
            (ps,) = proj([base + 16], hT, hT_r)

            rope(ps, [(slice(0, 128), kdup[li][:, tok0:tok0 + TB], kdup_r[li][blk])])
            if STG <= 1.4:
                return
            (ps,) = proj([base + 17], hT, hT_r)
            vw = T32.next()
            op("act", lambda e: e.activation(out=vw.t[:, :], in_=ps.t[:, :], func=AF.Copy), [ps.res], [vw.res])
            pst = PS.next()
            for tt in range(4):
                op("pe", lambda e: e.transpose(pst.t[:, tt * 128:(tt + 1) * 128], vw.t[:, tt * 128:(tt + 1) * 128], identF),
                   [vw.res, cst.res], [pst.res])
            pv = pst.t[:, :].rearrange("p (a b) -> p a b", a=4)
            op("act", lambda e: e.activation(out=vaug[li][:, gi0:gi0 + 4, 0:64], in_=pv[:, :, 0:64], func=AF.Copy), [pst.res], [vaug_r[li][blk]])
            op("dve", lambda e: e.tensor_scalar(out=witok.t[:, :, :], in0=pv[:, :, 64:72], scalar1=float(8 ** -0.5 * 64 ** -0.5), scalar2=None,
                                                op0=ALU.mult), [pst.res], [witok.res])
            if STG <= 1.6:
                return
            for c in range(4):
                (ps,) = proj([base + 18 + c], hT, hT_r)

                rope(ps, [(slice(0, 64), qiz.t[0:64, 2 * c, :], qiz_r[2 * c]), (slice(64, 128), qiz.t[64:128, 2 * c + 1, :], qiz_r[2 * c + 1])])
            (ps,) = proj([base + 22], hT, hT_r)

            rope(ps, [(slice(0, 128), kidup[li][:, tok0:tok0 + TB], kidup_r[li][blk])])
            for c in range(4):
                (ps,) = proj([base + 23 + c], hT, hT_r)
                op("act", lambda e: e.activation(out=sgA[:, c, :], in_=ps.t[:, :], func=AF.Silu), [ps.res], [sgA_r[c]])

            if STG <= 2:
                return
            def conv_module():
                s1 = PS.next()
                s2 = PS.next()
                for cc in range(4):
                    dg = dgs[cc]
                    ps = PS.next()
                    for j in range(31):
                        op("pe", lambda e: e.matmul(ps.t[:, :], dg.t[:, j, :], cglu[:, cc, j:j + TB], start=(j == 0), stop=(j == 30)),
                           [dg.res, cglu_r[cc]], [ps.res])
                    if cc + 2 < 4:
                        dg_load(cc + 2)
                    bcol = par.t[:, P_CBB + li * 4 + cc: P_CBB + li * 4 + cc + 1]
                    op("act", lambda e: e.activation(out=xc[:, cc, :], in_=ps.t[:, :], func=AF.Identity, bias=bcol, scale=1.0),
                       [ps.res, par.res], [xc_r[cc]])
                    sq = T16.next()
                    op("act", lambda e: e.activation(out=sq.t[:, :], in_=ps.t[:, :], func=AF.Square, bias=bcol, scale=1.0),
                       [ps.res, par.res], [sq.res])
                    op("pe", lambda e: e.matmul(s1.t[:, :], onesB, xc[:, cc, :], start=(cc == 0), stop=(cc == 3)), [xc_r[cc], cb.res], [s1.res])
                    op("pe", lambda e: e.matmul(s2.t[:, :], onesB, sq.t[:, :], start=(cc == 0), stop=(cc == 3)), [sq.res, cb.res], [s2.res])
                mean = T32.next()
                op("dve", lambda e: e.tensor_scalar(out=mean.t[:, :], in0=s1.t[:, :], scalar1=1.0 / 512, scalar2=None, op0=ALU.mult), [s1.res], [mean.res])
                msq = T32.next()
                op("pool", lambda e: e.tensor_tensor(out=msq.t[:, :], in0=mean.t[:, :], in1=mean.t[:, :], op=ALU.mult), [mean.res], [msq.res])
                var = T32.next()
                op("dve", lambda e: e.scalar_tensor_tensor(out=var.t[:, :], in0=s2.t[:, :], scalar=1.0 / 512, in1=msq.t[:, :],
                                                           op0=ALU.mult, op1=ALU.subtract), [s2.res, msq.res], [var.res])
                op("dve", lambda e: e.tensor_scalar(out=var.t[:, :], in0=var.t[:, :], scalar1=0.0, scalar2=None, op0=ALU.max), [var.res], [var.res])
                sd = T32.next()
                op("act", lambda e: e.activation(out=sd.t[:, :], in_=var.t[:, :], func=AF.Sqrt, scale=1.0, bias=par.t[:, P_EPS:P_EPS + 1]),
                   [var.res, par.res], [sd.res])
                rs = T32.next()
                op("dve", lambda e: e.reciprocal(out=rs.t[:, :], in_=sd.t[:, :]), [sd.res], [rs.res])
                for cc in range(4):
                    d = T32.next()
                    op("dve", lambda e: e.tensor_tensor(out=d.t[:, :], in0=xc[:, cc, :], in1=mean.t[:, :], op=ALU.subtract), [xc_r[cc], mean.res], [d.res])
                    op("dve", lambda e: e.tensor_tensor(out=d.t[:, :], in0=d.t[:, :], in1=rs.t[:, :], op=ALU.mult), [d.res, rs.res], [d.res])
                    sl = T16.next()
                    gcol = par.t[:, P_LNG + li * 4 + cc: P_LNG + li * 4 + cc + 1]
                    bcol2 = par.t[:, P_LNB + li * 4 + cc: P_LNB + li * 4 + cc + 1]
                    op("act", lambda e: e.activation(out=sl.t[:, :], in_=d.t[:, :], func=AF.Silu, scale=gcol, bias=bcol2), [d.res, par.res], [sl.res])
                    op("pool", lambda e: e.tensor_tensor(out=yT[:, 4 + cc, :], in0=sl.t[:, :], in1=sgB[:, cc, :], op=ALU.mult),
                       [sl.res, sgB_r[cc]], [yT_r[4 + cc]])


            kd = kdup[li]
            kid = kidup[li]
            junk = xc[:, :, :].rearrange("p a b -> p (a b)")
            krs = kidup_r[li][0:blk + 1]
            kr = kdup_r[li][0:blk + 1]
            vr = vaug_r[li][0:blk + 1]
            ctx = {}

            def scores(qt):
                gi = gi0 + qt
                nk = gi + 1
                NKC = nk * 128
                qc = slice(qt * 128, (qt + 1) * 128)
                acc = ACC.next()
                dgw = DW.next()
                for h in range(8):
                    op("pool", lambda e: e.tensor_scalar(out=dgw.t[:, h, :], in0=identB, scalar1=witok.t[:, qt, h:h + 1], scalar2=0.0,
                                                         op0=ALU.mult, op1=ALU.add), [cb.res, witok.res], [dgw.res])
                nkb = (nk + 3) // 4
                steps = [(kb, h) for kb in range(nkb) for h in range(8)]
                abank = {}
                pend = []

                def dots(kb, h):
                    c0 = kb * 512
                    n = min(NKC, c0 + 512) - c0
                    psd = PD.next()
                    op("pe", lambda e: e.matmul(psd.t[:, 0:n], qiz.t[:, h, qc], kid[:, c0:c0 + n], start=True, stop=True),
                       [qiz_r[h]] + krs, [psd.res])
                    r = R16.next()
                    op("act", lambda e: e.activation(out=r.t[:, 0:n], in_=psd.t[:, 0:n], func=AF.Relu), [psd.res], [r.res])
                    return r, n

                def wsum(kb, h, r, n):
                    if h == 0:
                        abank[kb] = PA.next()
                    a = abank[kb]
                    op("pe", lambda e: e.matmul(a.t[:, 0:n], dgw.t[:, h, :], r.t[:, 0:n], start=(h == 0), stop=(h == 7)), [dgw.res, r.res], [a.res])
                    if h == 7:
                        c0 = kb * 512
                        op("act", lambda e: e.activation(out=acc.t[:, c0:c0 + n], in_=a.t[:, 0:n], func=AF.Copy), [a.res], [acc.res])

                for (kb, h) in steps:
                    pend.append((kb, h) + dots(kb, h))
                    if len(pend) > 1:
                        wsum(*pend.pop(0))
                while pend:
                    wsum(*pend.pop(0))
                ctx[qt] = {"acc": acc, "nm": None}

            def bisect_multi(qts, inject):
                st = []
                for qt in qts:
                    gi = gi0 + qt
                    st.append(dict(qt=qt, gi=gi, NKC=(gi + 1) * 128, acc=ctx[qt]["acc"], smt=SM.next(), nm=NM.next()))
                for d_ in st:
                    if d_["gi"] >= 2:
                        op("dve", lambda e: e.tensor_reduce(out=d_["smt"].t[:, 1:2], in_=d_["acc"].t[:, 0:d_["NKC"]], axis=AX.X, op=ALU.max,
                                                            apply_absolute_value=True), [d_["acc"].res], [d_["smt"].res])
                for d_ in st:
                    g0 = d_["gi"] * 128
                    op("pool", lambda e: e.tensor_tensor(out=d_["acc"].t[:, g0:g0 + 128], in0=d_["acc"].t[:, g0:g0 + 128],
                                                         in1=causneg, op=ALU.add), [d_["acc"].res, cst.res, d_["smt"].res], [d_["acc"].res])
                act_ = [d_ for d_ in st if d_["gi"] >= 2]
                for d_ in st:
                    t_ = d_["smt"].t
                    if d_["gi"] >= 2:
                        op("dve", lambda e: e.tensor_scalar(out=t_[:, 0:1], in0=t_[:, 1:2], scalar1=-1.0, scalar2=None, op0=ALU.mult), [d_["smt"].res], [d_["smt"].res])
                        op("dve", lambda e: e.tensor_scalar(out=t_[:, 2:3], in0=t_[:, 1:2], scalar1=2.0002, scalar2=1e-20, op0=ALU.mult, op1=ALU.add),
                           [d_["smt"].res], [d_["smt"].res])
                        op("dve", lambda e: e.tensor_scalar(out=t_[:, 8:8 + NIT], in0=par.t[:, P_CK:P_CK + NIT], scalar1=t_[:, 2:3], scalar2=None, op0=ALU.mult),
                           [d_["smt"].res, par.res], [d_["smt"].res])
                        op("dve", lambda e: e.tensor_tensor(out=t_[:, 0:1], in0=t_[:, 0:1], in1=t_[:, 8:9], op=ALU.add), [d_["smt"].res], [d_["smt"].res])
                    else:
                        op("dve", lambda e: e.memset(t_[:, 0:1], -1e29), [], [d_["smt"].res])
                for k in range(NIT):
                    last = (k == NIT - 1)
                    for d_ in act_:
                        t_ = d_["smt"].t
                        n_ = d_["NKC"]
                        op("dve", lambda e: e.tensor_scalar(out=d_["nm"].t[:, 0:n_], in0=d_["acc"].t[:, 0:n_], scalar1=t_[:, 0:1], scalar2=None,
                                                            op0=ALU.is_ge, op1=ALU.add, accum_out=t_[:, 3:4]), [d_["acc"].res, d_["smt"].res],
                           [d_["nm"].res, d_["smt"].res])
                    for d_ in act_:
                        t_ = d_["smt"].t
                        op("dve", lambda e: e.tensor_scalar(out=t_[:, 4:5], in0=t_[:, 3:4], scalar1=TOPK - 0.5, scalar2=(-1.0 if last else -0.5),
                                                            op0=ALU.is_ge, op1=ALU.add), [d_["smt"].res], [d_["smt"].res])
                    for d_ in act_:
                        t_ = d_["smt"].t
                        op("dve", lambda e: e.scalar_tensor_tensor(out=t_[:, 0:1], in0=t_[:, 4:5], scalar=t_[:, 8 + k:9 + k], in1=t_[:, 0:1],
                                                                   op0=ALU.mult, op1=ALU.add), [d_["smt"].res], [d_["smt"].res])
                    if k == NIT // 2 and inject is not None and act_:
                        inject()
                        inject = None
                if inject is not None:
                    inject()
                for d_ in st:
                    n_ = d_["NKC"]
                    op("dve", lambda e: e.tensor_scalar(out=d_["nm"].t[:, 0:n_], in0=d_["acc"].t[:, 0:n_], scalar1=d_["smt"].t[:, 0:1], scalar2=-1.0,
                                                        op0=ALU.is_ge, op1=ALU.add), [d_["acc"].res, d_["smt"].res], [d_["nm"].res])
                    ctx[d_["qt"]]["nm"] = d_["nm"]

            def attn_pe(qt):
                gi = gi0 + qt
                nk = gi + 1
                qc = slice(qt * 128, (qt + 1) * 128)
                nm = ctx[qt]["nm"]
                nkb = (nk + 3) // 4
                groups = [(h, jb) for h in range(8) for jb in range(nkb)]

                def logits(h, jb):
                    js = list(range(jb * 4, min(nk, jb * 4 + 4)))
                    lg = PL.next()
                    for jj, j in enumerate(js):
                        op("pe", lambda e: e.matmul(lg.t[:, jj * 128:(jj + 1) * 128], kd[:, j * 128:(j + 1) * 128], qz.t[:, h, qc], start=True, stop=False),
                           [qz_r[h]] + kr, [lg.res])
                        op("pe", lambda e: e.matmul(lg.t[:, jj * 128:(jj + 1) * 128], nm.t[:, j * 128:(j + 1) * 128], bigI, start=False, stop=True),
                           [nm.res, cb.res], [lg.res])
                    pt = PT.next()
                    n = len(js) * 128
                    op("act", lambda e: e.activation(out=pt.t[:, 0:n], in_=lg.t[:, 0:n], func=AF.Exp, scale=0.125), [lg.res], [pt.res])
                    return pt, js

                def pv_acc(h, jb, pt, js):
                    bank = PO[h // 4]
                    hh = h % 4
                    for jj, j in enumerate(js):
                        op("pe", lambda e: e.matmul(bank.t[:, hh * 65:hh * 65 + 65], pt.t[:, jj * 128:(jj + 1) * 128], vaug[li][:, j, :],
                                                    start=(j == 0), stop=(j == nk - 1)), [pt.res] + vr, [bank.res])

                pend = []
                for (h, jb) in groups:
                    pend.append((h, jb) + logits(h, jb))
                    if len(pend) > 1:
                        pv_acc(*pend.pop(0))
                while pend:
                    pv_acc(*pend.pop(0))

            def normalize(qt):
                qc = slice(qt * 128, (qt + 1) * 128)
                for b in range(2):
                    ov = PO[b].t[:, 0:260].rearrange("p (h d) -> p h d", d=65)
                    op("dve", lambda e: e.reciprocal(out=rinv.t[:, 4 * b:4 * b + 4], in_=ov[:, :, 64]), [PO[b].res], [rinv.res])
                    for hh in range(4):
                        h = 4 * b + hh
                        op("dve", lambda e: e.tensor_scalar(out=atok.t[:, h, :], in0=ov[:, hh, 0:64], scalar1=rinv.t[:, h:h + 1], scalar2=None,
                                                            op0=ALU.mult), [PO[b].res, rinv.res], [atok.res])
                ptr = PL.next()
                pvw = ptr.t[:, :].bitcast(BF16)
                for c in range(4):
                    op("pe", lambda e: e.transpose(pvw[:, c * 128:(c + 1) * 128], atok.t[:, 2 * c:2 * c + 2, :].rearrange("p a b -> p (a b)"), identB),
                       [atok.res, cb.res], [ptr.res])
                op("dve", lambda e: e.tensor_tensor(out=yT[:, 0:4, qc], in0=pvw[:, 0:512].rearrange("p (a b) -> p a b", a=4), in1=sgA[:, 0:4, qc],
                                                    op=ALU.mult), [ptr.res] + sgA_r, yT_r[0:4])

            scores(0)
            scores(1)
            bisect_multi([0, 1], None)
            conv_module()
            scores(2)
            scores(3)
            attn_pe(0)
            bisect_multi([2, 3], lambda: normalize(0))
            attn_pe(1)
            normalize(1)
            attn_pe(2)
            normalize(2)
            attn_pe(3)
            normalize(3)
            if STG <= 5:
                return
            out_proj(base + 27)

        def odd_layer(l, blk):
            li = l // 2
            base = LBASE[l]
            norm_to_hT(l)
            for m in range(8):
                dg = DG3.next()
                for j in range(3):
                    wcol = P_CCW + (li * 8 + m) * 3 + j
                    op("pool", lambda e: e.tensor_scalar(out=dg.t[:, j, :], in0=identB, scalar1=par.t[:, wcol:wcol + 1], scalar2=0.0,
                                                         op0=ALU.mult, op1=ALU.add), [cb.res, par.res], [dg.res])
                ps_cg, ps_x, ps_g, ps_b = proj([base + 4 * m + i for i in range(4)], hT, hT_r)
                z = ZB.next()
                if blk == 0:
                    op("pool", lambda e: e.memset(z.t[:, 0:2], 0.0), [], [z.res])
                else:
                    op("pool", lambda e: e.tensor_copy(out=z.t[:, 0:2], in_=zhal.t[:, li, m, :]), [zhal.res], [z.res])
                cg = T32.next()
                op("act", lambda e: e.activation(out=cg.t[:, :], in_=ps_cg.t[:, :], func=AF.Copy), [ps_cg.res], [cg.res])
                op("dve", lambda e: e.tensor_tensor(out=z.t[:, 2:2 + TB], in0=ps_x.t[:, :], in1=cg.t[:, :], op=ALU.mult), [ps_x.res, cg.res], [z.res])
                op("pool", lambda e: e.tensor_copy(out=zhal.t[:, li, m, :], in_=z.t[:, TB:TB + 2]), [z.res], [zhal.res])
                pcv = PS.next()
                for j in range(3):
                    op("pe", lambda e: e.matmul(pcv.t[:, :], dg.t[:, j, :], z.t[:, j:j + TB], start=(j == 0), stop=(j == 2)), [dg.res, z.res], [pcv.res])
                sg = T32.next()
                op("act", lambda e: e.activation(out=sg.t[:, :], in_=ps_g.t[:, :], func=AF.Silu), [ps_g.res], [sg.res])
                t = T32.next()
                op("dve", lambda e: e.tensor_tensor(out=t.t[:, :], in0=pcv.t[:, :], in1=sg.t[:, :], op=ALU.mult), [pcv.res, sg.res], [t.res])
                op("dve", lambda e: e.tensor_tensor(out=yT[:, m, :], in0=ps_b.t[:, :], in1=t.t[:, :], op=ALU.mult), [ps_b.res, t.res], [yT_r[m]])
            out_proj(base + 32)

        def final_out(s, tok0):
            norm_stats()
            for kc in range(8):
                g = par.t[:, P_GF + kc: P_GF + kc + 1]
                op("dve", lambda e: e.scalar_tensor_tensor(out=xT[:, kc, :], in0=xT[:, kc, :], scalar=g, in1=rstd.t[:, :],
                                                           op0=ALU.mult, op1=ALU.mult), [xT_r[kc], rstd.res, par.res], [xT_r[kc]])
            for tt in range(4):
                og = XIN.next()
                for half in range(2):
                    ps = PS.next()
                    for j in range(4):
                        kc = half * 4 + j
                        op("pe", lambda e: e.transpose(ps.t[:, j * 128:(j + 1) * 128], xT[:, kc, tt * 128:(tt + 1) * 128], identF),
                           [xT_r[kc], cst.res], [ps.res])
                    op("act", lambda e: e.activation(out=og.t[:, half * 512:(half + 1) * 512], in_=ps.t[:, :], func=AF.Copy), [ps.res], [og.res])
                S.dma("act", out_d[s, tok0 + tt * 128: tok0 + (tt + 1) * 128, :], og.t[:, :], [og.res], [])

        for s in range(NS):
            for blk in range(NBLK):
                tok0 = blk * TB
                load_x(s, tok0)
                rope_tables(s, tok0)
                if s == 0 and blk == 0:
                    for _ in range(min(n_cast, 40)):
                        prepass_one()
                    w_load_more()
                for l in range(NL):
                    if l % 2 == 0:
                        even_layer(l, blk)
                    else:
                        odd_layer(l, blk)
                final_out(s, tok0)
        S.finish()
        print(f"[build] instructions={S.n_ins} waits={S.n_wait} dmas={S.dma_i} per-engine=" + str({n: e.cnt for n, e in S.engs.items()}), flush=True)
    return nc


def _chunk(cols):
    return np.ascontiguousarray(cols.reshape(8, 128, 128).transpose(1, 0, 2)).reshape(128, 1024)


def prep_weights(w_in_even, w_out_even, w_in_odd, w_out_odd):
    wall = np.zeros((NCH, 128, 1024), np.float32)
    z64 = np.zeros((1024, 56), np.float32)
    for i in range(2):
        We = np.asarray(w_in_even[i], np.float32)
        q, k, v = We[:, 0:512], We[:, 512:576], We[:, 576:640]
        qi, ki, wi = We[:, 640:1152], We[:, 1152:1216], We[:, 1216:1224]
        ga, lin, gg, gb = We[:, 1224:1736], We[:, 1736:2248], We[:, 2248:2760], We[:, 2760:3272]
        ch = []
        for c in range(4):
            ch.append(lin[:, c * 128:(c + 1) * 128])
            ch.append(gg[:, c * 128:(c + 1) * 128])
        for c in range(4):
            ch.append(gb[:, c * 128:(c + 1) * 128])
        for c in range(4):
            ch.append(q[:, c * 128:(c + 1) * 128])
        ch.append(np.concatenate([k, k], axis=1))
        ch.append(np.concatenate([v, wi, z64], axis=1))
        for c in range(4):
            ch.append(qi[:, c * 128:(c + 1) * 128])
        ch.append(np.concatenate([ki, ki], axis=1))
        for c in range(4):
            ch.append(ga[:, c * 128:(c + 1) * 128])
        Wo = np.asarray(w_out_even[i], np.float32)
        for m in range(8):
            ch.append(Wo[:, m * 128:(m + 1) * 128])
        assert len(ch) == NCH_E
        for j, cm in enumerate(ch):
            wall[LBASE[2 * i] + j] = _chunk(cm)
        Wi = np.asarray(w_in_odd[i], np.float32)
        bg, cg, xi, gt = Wi[:, 0:1024], Wi[:, 1024:2048], Wi[:, 2048:3072], Wi[:, 3072:4096]
        ch = []
        for m in range(8):
            sl = slice(m * 128, (m + 1) * 128)
            ch += [cg[:, sl], xi[:, sl], gt[:, sl], bg[:, sl]]
        Wo = np.asarray(w_out_odd[i], np.float32)
        for m in range(8):
            ch.append(Wo[:, m * 128:(m + 1) * 128])
        assert len(ch) == NCH_O
        for j, cm in enumerate(ch):
            wall[LBASE[2 * i + 1] + j] = _chunk(cm)
    return wall


def prep_params(norm_g, final_g, conv_b_w, conv_b_bias, conv_ln_g, conv_ln_b, conv_c_w):
    par = np.zeros((128, NPAR), np.float32)
    par[:, P_G0:P_G0 + 32] = np.asarray(norm_g, np.float32).reshape(4, 8, 128).transpose(2, 0, 1).reshape(128, 32)
    par[:, P_GF:P_GF + 8] = np.asarray(final_g, np.float32).reshape(8, 128).T
    par[:, P_CBW:P_CBW + 248] = np.asarray(conv_b_w, np.float32).reshape(2, 31, 4, 128).transpose(3, 0, 2, 1).reshape(128, 248)
    par[:, P_CBB:P_CBB + 8] = np.asarray(conv_b_bias, np.float32).reshape(2, 4, 128).transpose(2, 0, 1).reshape(128, 8)
    par[:, P_LNG:P_LNG + 8] = np.asarray(conv_ln_g, np.float32).reshape(2, 4, 128).transpose(2, 0, 1).reshape(128, 8)
    par[:, P_LNB:P_LNB + 8] = np.asarray(conv_ln_b, np.float32).reshape(2, 4, 128).transpose(2, 0, 1).reshape(128, 8)
    par[:, P_CCW:P_CCW + 48] = np.asarray(conv_c_w, np.float32).reshape(2, 3, 8, 128).transpose(3, 0, 2, 1).reshape(128, 48)
    half = 32
    inv = (np.float32(10000.0) ** (-(np.arange(half, dtype=np.float32)) / np.float32(half))).astype(np.float32)
    par[:, P_INV] = inv[np.arange(128) % 32]
    par[:, P_EPS] = 1e-6
    par[:, P_CK:P_CK + NIT] = (0.5 ** (np.arange(NIT, dtype=np.float64) + 1)).astype(np.float32)[None, :]
    return par


def prep_consts():
    cst = np.zeros((128, 384), np.float32)
    cst[:, 0:128] = np.eye(128, dtype=np.float32)
    Rm = np.zeros((128, 128), np.float32)
    for hb in (0, 64):
        for d2 in range(64):
            if d2 < 32:
                Rm[hb + d2 + 32, hb + d2] = -1.0
            else:
                Rm[hb + d2 - 32, hb + d2] = 1.0
    cst[:, 128:256] = Rm
    q = np.arange(128)[:, None]
    s = np.arange(128)[None, :]
    cst[:, 256:384] = np.where(s <= q, 0.0, -1e30).astype(np.float32)
    return cst


_CACHE = {}
N_LAUNCH = 1


def run_cores(x, positions, wall, par, cst, n_cores, NS, NL):
    key = (NS, NL)
    if key not in _CACHE:
        _CACHE[key] = build_program(NS, NL)
    nc = _CACHE[key]
    in_maps = []
    for c in range(n_cores):
        in_maps.append({"x": np.ascontiguousarray(x[c * NS:(c + 1) * NS]),
                        "pos": np.ascontiguousarray(positions[c * NS:(c + 1) * NS]).astype(np.int32),
                        "wall": wall, "par": par, "cst": cst})
    res = run_bass_kernel_spmd(nc, in_maps, core_ids=list(range(n_cores)))
    return np.concatenate([r["out"] for r in res.results], axis=0)


def kernel(x, positions, norm_g, w_in_even, w_out_even, conv_b_w, conv_b_bias,
           conv_ln_g, conv_ln_b, w_in_odd, conv_c_w, w_out_odd, final_g):
    x = np.asarray(x, np.float32)
    positions = np.asarray(positions)
    wall = prep_weights(w_in_even, w_out_even, w_in_odd, w_out_odd)
    par = prep_params(norm_g, final_g, conv_b_w, conv_b_bias, conv_ln_g, conv_ln_b, conv_c_w)
    cst = prep_consts()
    n_cores = 8
    n_launch = N_LAUNCH
    NS = x.shape[0] // (n_cores * n_launch)
    outs = []
    per = n_cores * NS
    for i in range(n_launch):
        outs.append(run_cores(x[i * per:(i + 1) * per], positions[i * per:(i + 1) * per], wall, par, cst, n_cores, NS, 4))
    return np.concatenate(outs, axis=0).astype(np.float32)
```

```python
import numpy as np
import concourse.bass as bass
import concourse.mybir as mybir
from concourse.bass_utils import run_bass_kernel_spmd
from contextlib import ExitStack

dt = mybir.dt
F32, BF16, I32 = dt.float32, dt.bfloat16, dt.int32
AF = mybir.ActivationFunctionType
ALU = mybir.AluOpType
AX = mybir.AxisListType

import os as _os
SEM_LIMIT = int(_os.environ.get("MK_SEMLIM", 1000000))


class Res:
    __slots__ = ("name", "w", "r", "excl")

    def __init__(self, name, excl=False):
        self.name = name
        self.w = None
        self.r = {}
        self.excl = excl


class _Eng:
    def __init__(self, name, h, sems):
        self.name = name
        self.h = h
        self.sems = sems
        self.si = 0
        self.cnt = 0
        self.seen = {}
        self.last = None


class Sched:
    def __init__(self, nc, stack, n_eng_sems=14, n_dma_sems=24):
        self.nc = nc
        self.engs = {}
        hs = {"pe": nc.tensor, "act": nc.scalar, "dve": nc.vector, "pool": nc.gpsimd, "sp": nc.sync}
        for n, h in hs.items():
            sems = [stack.enter_context(nc.semaphore(f"s_{n}_{i}")) for i in range(n_eng_sems if n != "sp" else 1)]
            self.engs[n] = _Eng(n, h, sems)
        self.dsems = [stack.enter_context(nc.semaphore(f"s_dma_{i}")) for i in range(n_dma_sems)]
        self.dma_i = 0
        self.dma_tokens = []
        self.n_wait = 0
        self.n_ins = 0

    def res(self, name):
        return Res(name)

    def _wait(self, E, tok):
        sem, val, en = tok
        if E.seen.get(id(sem), 0) >= val:
            return
        E.h.wait_ge(sem, val)
        E.seen[id(sem)] = val
        self.n_wait += 1

    def _deps(self, E, en, reads, writes):
        for r in reads:
            if r.w is not None:
                self._wait(E, r.w)
            if r.excl:
                for k, t in r.r.items():
                    if k != en:
                        self._wait(E, t)
        for w in writes:
            if w.w is not None and not (en == "pe" and w.w[2] == "pe"):
                self._wait(E, w.w)
            for t in w.r.values():
                self._wait(E, t)

    def _record(self, tok, key, reads, writes):
        for r in reads:
            r.r[key] = tok
        for w in writes:
            w.w = tok
            w.r = {}

    def op(self, en, fn, reads=(), writes=()):
        E = self.engs[en]
        self._deps(E, en, reads, writes)
        ins = fn(E.h)
        if E.cnt >= SEM_LIMIT:
            E.si += 1
            E.cnt = 0
        sem = E.sems[E.si]
        E.cnt += 1
        ins.then_inc(sem, 1)
        tok = (sem, E.cnt, en)
        E.last = tok
        self.n_ins += 1
        self._record(tok, en, reads, writes)
        return tok

    def dma(self, qn, out, in_, reads=(), writes=()):
        E = self.engs[qn]
        self._deps(E, qn, reads, writes)
        i = self.dma_i
        self.dma_i += 1
        nd = len(self.dsems)
        sem = self.dsems[i % nd]
        val = 16 * (i // nd + 1)
        if i >= nd:
            self._wait(E, (sem, val - 16, "dma"))
        ins = E.h.dma_start(out=out, in_=in_)
        ins.then_inc(sem, 16)
        tok = (sem, val, "dma")
        self.dma_tokens.append(tok)
        self.n_ins += 1
        self._record(tok, ("dma", i), reads, writes)
        return tok

    def finish(self):
        E = self.engs["sp"]
        for tok in self.dma_tokens[-len(self.dsems):]:
            self._wait(E, tok)
        for n, e in self.engs.items():
            if e.last is not None:
                self._wait(E, e.last)


D = 1024
S_LEN = 2048
TB = 512
NBLK = S_LEN // TB
NCH_E = 35
NCH_O = 40
LBASE = [0, 35, 75, 110]
NCH = 150
NW = 6
NIT = 16
TOPK = 256
P_G0, P_GF, P_CBW, P_CBB, P_LNG, P_LNB, P_CCW, P_INV, P_EPS, P_CK = 0, 32, 40, 288, 296, 304, 312, 360, 361, 368
NPAR = 400
TWO_PI = 6.283185307179586
PI = 3.141592653589793


class _Tile:
    __slots__ = ("t", "res")

    def __init__(self, t, res):
        self.t = t
        self.res = res


class _Rot:
    def __init__(self, tiles):
        self.tiles = tiles
        self.i = 0

    def next(self):
        t = self.tiles[self.i % len(self.tiles)]
        self.i += 1
        return t


def build_program(NS, NL):
    nc = bass.Bass("TRN2", target_bir_lowering=False)
    x_d = nc.dram_tensor("x", [NS, S_LEN, D], F32, kind="ExternalInput").ap()
    pos_d = nc.dram_tensor("pos", [NS, S_LEN], I32, kind="ExternalInput").ap()
    wall_d = nc.dram_tensor("wall", [NCH, 128, 1024], F32, kind="ExternalInput").ap()
    par_d = nc.dram_tensor("par", [128, NPAR], F32, kind="ExternalInput").ap()
    cst_d = nc.dram_tensor("cst", [128, 384], F32, kind="ExternalInput").ap()
    out_d = nc.dram_tensor("out", [NS, S_LEN, D], F32, kind="ExternalOutput").ap()
    wbf_d = nc.dram_tensor("wbf", [NCH, 128, 1024], BF16, kind="Internal").ap()
    dgd_d = nc.dram_tensor("dgd", [8, 128, 31 * 128], BF16, kind="Internal").ap()

    with ExitStack() as st:
        S = Sched(nc, st)
        R = S.res

        def sb(name, shape, dtype):
            return nc.alloc_sbuf_tensor("sb_" + name, shape, dtype)

        def mk(name, shape, dtype):
            return _Tile(sb(name, shape, dtype), R(name))

        def mkrot(name, n, shape, dtype):
            return _Rot([mk(f"{name}{i}", shape, dtype) for i in range(n)])

        par = mk("par", [128, NPAR], F32)
        cst = mk("cst", [128, 384], F32)
        identF = cst.t[:, 0:128]
        causneg = cst.t[:, 256:384]
        cb = mk("cb", [128, 4, 128], BF16)
        identB, onesB, bigI, RmB = cb.t[:, 0, :], cb.t[:, 1, :], cb.t[:, 2, :], cb.t[:, 3, :]
        xT = sb("xT", [128, 8, TB], F32)
        xT_r = [R(f"xT{k}") for k in range(8)]
        hT = sb("hT", [128, 8, TB], BF16)
        hT_r = [R(f"hT{k}") for k in range(8)]
        yT = sb("yT", [128, 8, TB], BF16)
        yT_r = [R(f"yT{k}") for k in range(8)]
        rstd = mk("rstd", [128, TB], F32)
        cosT = mk("cosT", [128, TB], F32)
        sinT = mk("sinT", [128, TB], F32)
        kdup = [sb(f"kdup{i}", [128, S_LEN], BF16) for i in range(2)]
        kidup = [sb(f"kidup{i}", [128, S_LEN], BF16) for i in range(2)]
        kdup_r = [[R(f"kdup{i}_{b}") for b in range(NBLK)] for i in range(2)]
        kidup_r = [[R(f"kidup{i}_{b}") for b in range(NBLK)] for i in range(2)]
        vaug = [sb(f"vaug{i}", [128, 16, 65], BF16) for i in range(2)]
        vaug_r = [[R(f"vaug{i}_{b}") for b in range(NBLK)] for i in range(2)]
        qz = mk("qz", [128, 8, TB], BF16)
        qiz = mk("qiz", [128, 8, TB], BF16)
        qz_r = [R(f"qz{h}") for h in range(8)]
        qiz_r = [R(f"qiz{h}") for h in range(8)]
        sgA = sb("sgA", [128, 4, TB], BF16)
        sgA_r = [R(f"sgA{c}") for c in range(4)]
        sgB = sb("sgB", [128, 4, TB], BF16)
        sgB_r = [R(f"sgB{c}") for c in range(4)]
        cglu = sb("cglu", [128, 4, 30 + TB], BF16)
        cglu_r = [R(f"cglu{c}") for c in range(4)]
        chal = mk("chal", [128, 2, 4, 30], BF16)
        zhal = mk("zhal", [128, 2, 8, 2], BF16)
        xc = sb("xc", [128, 4, TB], BF16)
        xc_r = [R(f"xc{c}") for c in range(4)]
        witok = mk("witok", [128, 4, 8], F32)
        wring = mkrot("wr", NW, [128, 8, 128], BF16)
        DG = mkrot("dg", 2, [128, 31, 128], BF16)
        DG3 = mkrot("dg3", 2, [128, 3, 128], BF16)
        DW = mkrot("dgw", 4, [128, 8, 128], BF16)
        ZB = mkrot("zb", 2, [128, 2 + TB], BF16)
        ACC = mkrot("acc", 2, [128, S_LEN], F32)
        NM = mkrot("nm", 4, [128, S_LEN], BF16)
        R16 = mkrot("r16", 3, [128, 512], BF16)
        PT = mkrot("pt", 3, [128, 512], BF16)
        T32 = mkrot("t32", 8, [128, 512], F32)
        T16 = mkrot("t16", 4, [128, 512], BF16)
        XIN = mkrot("xin", 2, [128, 1024], F32)
        WST = mkrot("wst", 2, [128, 1024], BF16)
        atok = mk("atok", [128, 8, 64], BF16)
        SM = mkrot("sm", 2, [128, 32], F32)
        rinv = mk("rinv", [128, 8], F32)
        posi = mk("posi", [128, TB], I32)
        RT = T32

        banks = [_Tile(nc.alloc_psum_tensor(f"ps{i}", [128, 512], F32), Res(f"ps{i}", excl=True)) for i in range(8)]
        PS = _Rot(banks)
        PA = _Rot(banks[0:2])
        PD = _Rot(banks[2:4])
        PO = banks[4:6]
        PL = _Rot(banks[6:8])

        wbf_r = [R(f"wbf{c}") for c in range(NCH)]
        dgd_r = [R(f"dgd{c}") for c in range(8)]

        def op(en, fn, reads=(), writes=()):
            return S.op(en, fn, list(reads), list(writes))

        S.dma("sp", par.t[:, :], par_d, [], [par.res])
        S.dma("sp", cst.t[:, :], cst_d, [], [cst.res])
        n_cast = LBASE[NL] if NL < 4 else NCH
        cast_engs = ["act", "dve", "pool"]
        pp = {"next": 0}

        def prepass_one():
            c = pp["next"]
            if c >= n_cast:
                return
            pp["next"] += 1
            sf = XIN.next()
            sbf = WST.next()
            S.dma("sp", sf.t[:, :], wall_d[c], [], [sf.res])
            en = cast_engs[c % 3]
            if en == "act":
                op("act", lambda e: e.activation(out=sbf.t[:, :], in_=sf.t[:, :], func=AF.Copy), [sf.res], [sbf.res])
            else:
                op(en, lambda e: e.tensor_copy(out=sbf.t[:, :], in_=sf.t[:, :]), [sf.res], [sbf.res])
            S.dma("sp", wbf_d[c], sbf.t[:, :], [sbf.res], [wbf_r[c]])

        op("dve", lambda e: e.tensor_copy(out=identB, in_=identF), [cst.res], [cb.res])
        op("dve", lambda e: e.memset(onesB, 1.0), [], [cb.res])
        op("dve", lambda e: e.tensor_scalar(out=bigI, in0=identF, scalar1=32768.0, scalar2=None, op0=ALU.mult), [cst.res], [cb.res])
        op("dve", lambda e: e.tensor_copy(out=RmB, in_=cst.t[:, 128:256]), [cst.res], [cb.res])
        op("dve", lambda e: e.memset(qz.t[:, :, :], 0.0), [], [qz.res] + qz_r)
        op("dve", lambda e: e.memset(qiz.t[:, :, :], 0.0), [], [qiz.res] + qiz_r)
        for i in range(2):
            op("dve", lambda e: e.memset(vaug[i][:, :, :], 1.0), [], vaug_r[i])

        for i8 in range(8):
            dg = DG.next()
            for j in range(31):
                wcol = P_CBW + i8 * 31 + j
                op("pool", lambda e: e.tensor_scalar(out=dg.t[:, j, :], in0=identB, scalar1=par.t[:, wcol:wcol + 1], scalar2=0.0,
                                                     op0=ALU.mult, op1=ALU.add), [cb.res, par.res], [dg.res])
            S.dma("sp", dgd_d[i8], dg.t[:, :, :].rearrange("p a b -> p (a b)"), [dg.res], [dgd_r[i8]])

        import os
        STG = float(os.environ.get("MK_STAGE", 99))
        per_blk = LBASE[NL] if NL < 4 else NCH
        if STG < 6:
            per_blk = {1.0: 12, 1.2: 16, 1.4: 17, 1.6: 18}.get(STG, 27)
        total_w = NS * NBLK * per_blk
        ws = {"load": 0, "use": 0}

        def w_load_more():
            while ws["load"] < total_w and ws["load"] < ws["use"] + NW:
                i = ws["load"]
                ch = i % per_blk
                assert wbf_r[ch].w is not None, ch
                slot = wring.tiles[i % NW]
                S.dma("sp", slot.t[:, :, :], wbf_d[ch].rearrange("p (k m) -> p k m", k=8), [wbf_r[ch]], [slot.res])
                ws["load"] += 1

        def w_use(ch):
            i = ws["use"]
            assert i % per_blk == ch, (i, per_blk, ch)
            ws["use"] += 1
            return wring.tiles[i % NW]

        def proj(chs, rhs, rhs_r):
            outs = []
            for ch in chs:
                w = w_use(ch)
                ps = PS.next()
                for kc in range(8):
                    op("pe", lambda e: e.matmul(ps.t[:, :], w.t[:, kc, :], rhs[:, kc, :], start=(kc == 0), stop=(kc == 7)),
                       [w.res, rhs_r[kc]], [ps.res])
                w_load_more()
                prepass_one()
                outs.append(ps)
            return outs

        def load_x(s, tok0):
            for tt in range(4):
                xin = XIN.next()
                S.dma("sp", xin.t[:, :], x_d[s, tok0 + tt * 128: tok0 + (tt + 1) * 128, :], [], [xin.res])
                for half in range(2):
                    ps = PS.next()
                    for j in range(4):
                        kc = half * 4 + j
                        op("pe", lambda e: e.transpose(ps.t[:, j * 128:(j + 1) * 128], xin.t[:, kc * 128:(kc + 1) * 128], identF),
                           [xin.res, cst.res], [ps.res])
                    op("act", lambda e: e.activation(out=xT[:, half * 4:half * 4 + 4, tt * 128:(tt + 1) * 128],
                                                     in_=ps.t[:, :].rearrange("p (a b) -> p a b", a=4), func=AF.Copy),
                       [ps.res], xT_r[half * 4:half * 4 + 4])

        def rope_tables(s, tok0):
            S.dma("sp", posi.t[:, :], pos_d[s:s + 1, tok0:tok0 + TB].partition_broadcast(128), [], [posi.res])
            posf = RT.next()
            op("pool", lambda e: e.tensor_copy(out=posf.t[:, :], in_=posi.t[:, :]), [posi.res], [posf.res])
            ang = RT.next()
            op("pool", lambda e: e.tensor_scalar(out=ang.t[:, :], in0=posf.t[:, :], scalar1=par.t[:, P_INV:P_INV + 1], scalar2=0.0,
                                                 op0=ALU.mult, op1=ALU.add), [posf.res, par.res], [ang.res])
            for dst, shift in ((sinT, 0.0), (cosT, PI / 2)):
                t = RT.next()
                op("pool", lambda e: e.tensor_scalar(out=t.t[:, :], in0=ang.t[:, :], scalar1=1.0 / TWO_PI, scalar2=shift / TWO_PI,
                                                     op0=ALU.mult, op1=ALU.add), [ang.res], [t.res])
                rti = RT.next()
                rti_ap = rti.t[:, :].bitcast(I32)
                op("pool", lambda e: e.tensor_copy(out=rti_ap, in_=t.t[:, :]), [t.res], [rti.res])
                op("pool", lambda e: e.tensor_copy(out=t.t[:, :], in_=rti_ap), [rti.res], [t.res])
                r = RT.next()
                op("dve", lambda e: e.scalar_tensor_tensor(out=r.t[:, :], in0=t.t[:, :], scalar=-TWO_PI, in1=ang.t[:, :],
                                                           op0=ALU.mult, op1=ALU.add), [t.res, ang.res], [r.res])
                if shift != 0.0:
                    op("dve", lambda e: e.tensor_scalar(out=r.t[:, :], in0=r.t[:, :], scalar1=shift, scalar2=None, op0=ALU.add),
                       [r.res], [r.res])
                op("dve", lambda e: e.tensor_scalar(out=t.t[:, :], in0=r.t[:, :], scalar1=PI, scalar2=-TWO_PI, op0=ALU.is_gt, op1=ALU.mult),
                   [r.res], [t.res])
                op("dve", lambda e: e.tensor_tensor(out=r.t[:, :], in0=r.t[:, :], in1=t.t[:, :], op=ALU.add), [r.res, t.res], [r.res])
                op("dve", lambda e: e.tensor_scalar(out=t.t[:, :], in0=r.t[:, :], scalar1=-PI, scalar2=TWO_PI, op0=ALU.is_lt, op1=ALU.mult),
                   [r.res], [t.res])
                op("dve", lambda e: e.tensor_tensor(out=r.t[:, :], in0=r.t[:, :], in1=t.t[:, :], op=ALU.add), [r.res, t.res], [r.res])
                op("dve", lambda e: e.tensor_scalar(out=r.t[:, :], in0=r.t[:, :], scalar1=3.1415925, scalar2=-3.1415925, op0=ALU.min, op1=ALU.max),
                   [r.res], [r.res])
                op("act", lambda e: e.activation(out=dst.t[:, :], in_=r.t[:, :], func=AF.Sin), [r.res], [dst.res])

        def norm_stats():
            ps = PS.next()
            for kc in range(8):
                sq = T16.next()
                op("act", lambda e: e.activation(out=sq.t[:, :], in_=xT[:, kc, :], func=AF.Square), [xT_r[kc]], [sq.res])
                op("pe", lambda e: e.matmul(ps.t[:, :], onesB, sq.t[:, :], start=(kc == 0), stop=(kc == 7)), [sq.res, cb.res], [ps.res])
            sd = T32.next()
            op("act", lambda e: e.activation(out=sd.t[:, :], in_=ps.t[:, :], func=AF.Sqrt, scale=1.0 / D, bias=par.t[:, P_EPS:P_EPS + 1]),
               [ps.res, par.res], [sd.res])
            op("dve", lambda e: e.reciprocal(out=rstd.t[:, :], in_=sd.t[:, :]), [sd.res], [rstd.res])

        def norm_to_hT(l):
            norm_stats()
            for kc in range(8):
                g = par.t[:, P_G0 + l * 8 + kc: P_G0 + l * 8 + kc + 1]
                op("dve", lambda e: e.scalar_tensor_tensor(out=hT[:, kc, :], in0=xT[:, kc, :], scalar=g, in1=rstd.t[:, :],
                                                           op0=ALU.mult, op1=ALU.mult), [xT_r[kc], rstd.res, par.res], [hT_r[kc]])

        def rope(ps, dests):
            u1 = T16.next()
            op("dve", lambda e: e.tensor_tensor(out=u1.t[:, :], in0=ps.t[:, :], in1=cosT.t[:, :], op=ALU.mult), [ps.res, cosT.res], [u1.res])
            u2 = T16.next()
            op("dve", lambda e: e.tensor_tensor(out=u2.t[:, :], in0=ps.t[:, :], in1=sinT.t[:, :], op=ALU.mult), [ps.res, sinT.res], [u2.res])
            ps2 = PS.next()
            op("pe", lambda e: e.matmul(ps2.t[:, :], identB, u1.t[:, :], start=True, stop=False), [u1.res, cb.res], [ps2.res])
            op("pe", lambda e: e.matmul(ps2.t[:, :], RmB, u2.t[:, :], start=False, stop=True), [u2.res, cb.res], [ps2.res])
            for rows, out_ap, res in dests:
                op("act", lambda e: e.activation(out=out_ap, in_=ps2.t[rows, :], func=AF.Copy), [ps2.res], [res])

        def out_proj(base):
            for m in range(8):
                (ps,) = proj([base + m], yT, yT_r)
                op("dve", lambda e: e.tensor_tensor(out=xT[:, m, :], in0=ps.t[:, :], in1=xT[:, m, :], op=ALU.add), [ps.res, xT_r[m]], [xT_r[m]])

        def even_layer(l, blk):
            li = l // 2
            base = LBASE[l]
            gi0 = blk * 4
            tok0 = blk * TB
            norm_to_hT(l)
            dgs = {}

            def dg_load(cc):
                dg = DG.next()
                S.dma("sp", dg.t[:, :, :].rearrange("p a b -> p (a b)"), dgd_d[li * 4 + cc], [dgd_r[li * 4 + cc]], [dg.res])
                dgs[cc] = dg
            dg_load(0)
            dg_load(1)
            if blk == 0:
                op("pool", lambda e: e.memset(cglu[:, :, 0:30], 0.0), [], cglu_r)
            else:
                op("pool", lambda e: e.tensor_copy(out=cglu[:, :, 0:30], in_=chal.t[:, li, :, :]), [chal.res], cglu_r)
            for c in range(4):
                ps_l, ps_g = proj([base + 2 * c, base + 2 * c + 1], hT, hT_r)
                sig = T32.next()
                op("act", lambda e: e.activation(out=sig.t[:, :], in_=ps_g.t[:, :], func=AF.Sigmoid), [ps_g.res], [sig.res])
                op("dve", lambda e: e.tensor_tensor(out=cglu[:, c, 30:30 + TB], in0=ps_l.t[:, :], in1=sig.t[:, :], op=ALU.mult),
                   [ps_l.res, sig.res], [cglu_r[c]])
            op("pool", lambda e: e.tensor_copy(out=chal.t[:, li, :, :], in_=cglu[:, :, TB:TB + 30]), cglu_r, [chal.res])
            for c in range(4):
                (ps,) = proj([base + 8 + c], hT, hT_r)
                op("act", lambda e: e.activation(out=sgB[:, c, :], in_=ps.t[:, :], func=AF.Silu), [ps.res], [sgB_r[c]])
            if STG <= 1:
                return
            for c in range(4):
                (ps,) = proj([base + 12 + c], hT, hT_r)

                rope(ps, [(slice(0, 64), qz.t[0:64, 2 * c, :], qz_r[2 * c]), (slice(64, 128), qz.t[64:128, 2 * c + 1, :], qz_r[2 * c + 1])])
            if STG <= 1.2:
                return
            (ps,) = proj([base + 16], hT, hT_r)

            rope(ps, [(slice(0, 128), kdup[li][:, tok0:tok0 + TB], kdup_r[li][blk])])
            if STG <= 1.4:
                return
            (ps,) = proj([base + 17], hT, hT_r)
            vw = T32.next()
            op("act", lambda e: e.activation(out=vw.t[:, :], in_=ps.t[:, :], func=AF.Copy), [ps.res], [vw.res])
            pst = PS.next()
            for tt in range(4):
                op("pe", lambda e: e.transpose(pst.t[:, tt * 128:(tt + 1) * 128], vw.t[:, tt * 128:(tt + 1) * 128], identF),
                   [vw.res, cst.res], [pst.res])
            pv = pst.t[:, :].rearrange("p (a b) -> p a b", a=4)
            op("act", lambda e: e.activation(out=vaug[li][:, gi0:gi0 + 4, 0:64], in_=pv[:, :, 0:64], func=AF.Copy), [pst.res], [vaug_r[li][blk]])
            op("dve", lambda e: e.tensor_scalar(out=witok.t[:, :, :], in0=pv[:, :, 64:72], scalar1=float(8 ** -0.5 * 64 ** -0.5), scalar2=None,
                                                op0=ALU.mult), [pst.res], [witok.res])
            dgws = []
            for qt in range(4):
                dgw = DW.next()
                for h in range(8):
                    op("pool", lambda e: e.tensor_scalar(out=dgw.t[:, h, :], in0=identB, scalar1=witok.t[:, qt, h:h + 1], scalar2=0.0,
                                                         op0=ALU.mult, op1=ALU.add), [cb.res, witok.res], [dgw.res])
                dgws.append(dgw)
            if STG <= 1.6:
                return
            for c in range(4):
                (ps,) = proj([base + 18 + c], hT, hT_r)

                rope(ps, [(slice(0, 64), qiz.t[0:64, 2 * c, :], qiz_r[2 * c]), (slice(64, 128), qiz.t[64:128, 2 * c + 1, :], qiz_r[2 * c + 1])])
            (ps,) = proj([base + 22], hT, hT_r)

            rope(ps, [(slice(0, 128), kidup[li][:, tok0:tok0 + TB], kidup_r[li][blk])])
            for c in range(4):
                (ps,) = proj([base + 23 + c], hT, hT_r)
                op("act", lambda e: e.activation(out=sgA[:, c, :], in_=ps.t[:, :], func=AF.Silu), [ps.res], [sgA_r[c]])

            if STG <= 2:
                return
            def conv_module():
                s1 = PS.next()
                s2 = PS.next()
                for cc in range(4):
                    dg = dgs[cc]
                    ps = PS.next()
                    for j in range(31):
                        op("pe", lambda e: e.matmul(ps.t[:, :], dg.t[:, j, :], cglu[:, cc, j:j + TB], start=(j == 0), stop=(j == 30)),
                           [dg.res, cglu_r[cc]], [ps.res])
                    if cc + 2 < 4:
                        dg_load(cc + 2)
                    bcol = par.t[:, P_CBB + li * 4 + cc: P_CBB + li * 4 + cc + 1]
                    op("act", lambda e: e.activation(out=xc[:, cc, :], in_=ps.t[:, :], func=AF.Identity, bias=bcol, scale=1.0),
                       [ps.res, par.res], [xc_r[cc]])
                    sq = T16.next()
                    op("act", lambda e: e.activation(out=sq.t[:, :], in_=ps.t[:, :], func=AF.Square, bias=bcol, scale=1.0),
                       [ps.res, par.res], [sq.res])
                    op("pe", lambda e: e.matmul(s1.t[:, :], onesB, xc[:, cc, :], start=(cc == 0), stop=(cc == 3)), [xc_r[cc], cb.res], [s1.res])
                    op("pe", lambda e: e.matmul(s2.t[:, :], onesB, sq.t[:, :], start=(cc == 0), stop=(cc == 3)), [sq.res, cb.res], [s2.res])
                mean = T32.next()
                op("dve", lambda e: e.tensor_scalar(out=mean.t[:, :], in0=s1.t[:, :], scalar1=1.0 / 512, scalar2=None, op0=ALU.mult), [s1.res], [mean.res])
                msq = T32.next()
                op("pool", lambda e: e.tensor_tensor(out=msq.t[:, :], in0=mean.t[:, :], in1=mean.t[:, :], op=ALU.mult), [mean.res], [msq.res])
                var = T32.next()
                op("dve", lambda e: e.scalar_tensor_tensor(out=var.t[:, :], in0=s2.t[:, :], scalar=1.0 / 512, in1=msq.t[:, :],
                                                           op0=ALU.mult, op1=ALU.subtract), [s2.res, msq.res], [var.res])
                op("dve", lambda e: e.tensor_scalar(out=var.t[:, :], in0=var.t[:, :], scalar1=0.0, scalar2=None, op0=ALU.max), [var.res], [var.res])
                sd = T32.next()
                op("act", lambda e: e.activation(out=sd.t[:, :], in_=var.t[:, :], func=AF.Sqrt, scale=1.0, bias=par.t[:, P_EPS:P_EPS + 1]),
                   [var.res, par.res], [sd.res])
                rs = T32.next()
                op("dve", lambda e: e.reciprocal(out=rs.t[:, :], in_=sd.t[:, :]), [sd.res], [rs.res])
                for cc in range(4):
                    d = T32.next()
                    op("dve", lambda e: e.tensor_tensor(out=d.t[:, :], in0=xc[:, cc, :], in1=mean.t[:, :], op=ALU.subtract), [xc_r[cc], mean.res], [d.res])
                    op("dve", lambda e: e.tensor_tensor(out=d.t[:, :], in0=d.t[:, :], in1=rs.t[:, :], op=ALU.mult), [d.res, rs.res], [d.res])
                    sl = T16.next()
                    gcol = par.t[:, P_LNG + li * 4 + cc: P_LNG + li * 4 + cc + 1]
                    bcol2 = par.t[:, P_LNB + li * 4 + cc: P_LNB + li * 4 + cc + 1]
                    op("act", lambda e: e.activation(out=sl.t[:, :], in_=d.t[:, :], func=AF.Silu, scale=gcol, bias=bcol2), [d.res, par.res], [sl.res])
                    op("pool", lambda e: e.tensor_tensor(out=yT[:, 4 + cc, :], in0=sl.t[:, :], in1=sgB[:, cc, :], op=ALU.mult),
                       [sl.res, sgB_r[cc]], [yT_r[4 + cc]])


            kd = kdup[li]
            kid = kidup[li]
            junk = xc[:, :, :].rearrange("p a b -> p (a b)")
            krs = kidup_r[li][0:blk + 1]
            kr = kdup_r[li][0:blk + 1]
            vr = vaug_r[li][0:blk + 1]
            ctx = {}

            def scores(qt):
                gi = gi0 + qt
                nk = gi + 1
                NKC = nk * 128
                qc = slice(qt * 128, (qt + 1) * 128)
                acc = ACC.next()
                dgw = dgws[qt]
                nkb = (nk + 3) // 4
                steps = [(kb, h) for kb in range(nkb) for h in range(8)]
                abank = {}
                pend = []

                def dots(kb, h):
                    c0 = kb * 512
                    n = min(NKC, c0 + 512) - c0
                    psd = PD.next()
                    op("pe", lambda e: e.matmul(psd.t[:, 0:n], qiz.t[:, h, qc], kid[:, c0:c0 + n], start=True, stop=True),
                       [qiz_r[h]] + krs, [psd.res])
                    r = R16.next()
                    op("act", lambda e: e.activation(out=r.t[:, 0:n], in_=psd.t[:, 0:n], func=AF.Relu), [psd.res], [r.res])
                    return r, n

                def wsum(kb, h, r, n):
                    if h == 0:
                        abank[kb] = PA.next()
                    a = abank[kb]
                    op("pe", lambda e: e.matmul(a.t[:, 0:n], dgw.t[:, h, :], r.t[:, 0:n], start=(h == 0), stop=(h == 7)), [dgw.res, r.res], [a.res])
                    if h == 7:
                        c0 = kb * 512
                        op("act", lambda e: e.activation(out=acc.t[:, c0:c0 + n], in_=a.t[:, 0:n], func=AF.Copy), [a.res], [acc.res])

                for (kb, h) in steps:
                    pend.append((kb, h) + dots(kb, h))
                    if len(pend) > 1:
                        wsum(*pend.pop(0))
                while pend:
                    wsum(*pend.pop(0))
                ctx[qt] = {"acc": acc, "nm": None}

            def bisect_multi(qts, inject):
                st = []
                for qt in qts:
                    gi = gi0 + qt
                    st.append(dict(qt=qt, gi=gi, NKC=(gi + 1) * 128, acc=ctx[qt]["acc"], smt=SM.next(), nm=NM.next()))
                for d_ in st:
                    if d_["gi"] >= 2:
                        op("dve", lambda e: e.tensor_reduce(out=d_["smt"].t[:, 1:2], in_=d_["acc"].t[:, 0:d_["NKC"]], axis=AX.X, op=ALU.max,
                                                            apply_absolute_value=True), [d_["acc"].res], [d_["smt"].res])
                for d_ in st:
                    g0 = d_["gi"] * 128
                    op("pool", lambda e: e.tensor_tensor(out=d_["acc"].t[:, g0:g0 + 128], in0=d_["acc"].t[:, g0:g0 + 128],
                                                         in1=causneg, op=ALU.add), [d_["acc"].res, cst.res, d_["smt"].res], [d_["acc"].res])
                act_ = [d_ for d_ in st if d_["gi"] >= 2]
                for d_ in st:
                    t_ = d_["smt"].t
                    if d_["gi"] >= 2:
                        op("dve", lambda e: e.tensor_scalar(out=t_[:, 0:1], in0=t_[:, 1:2], scalar1=-1.0, scalar2=None, op0=ALU.mult), [d_["smt"].res], [d_["smt"].res])
                        op("dve", lambda e: e.tensor_scalar(out=t_[:, 2:3], in0=t_[:, 1:2], scalar1=2.0002, scalar2=1e-20, op0=ALU.mult, op1=ALU.add),
                           [d_["smt"].res], [d_["smt"].res])
                        op("dve", lambda e: e.tensor_scalar(out=t_[:, 8:8 + NIT], in0=par.t[:, P_CK:P_CK + NIT], scalar1=t_[:, 2:3], scalar2=None, op0=ALU.mult),
                           [d_["smt"].res, par.res], [d_["smt"].res])
                        op("dve", lambda e: e.tensor_tensor(out=t_[:, 0:1], in0=t_[:, 0:1], in1=t_[:, 8:9], op=ALU.add), [d_["smt"].res], [d_["smt"].res])
                    else:
                        op("dve", lambda e: e.memset(t_[:, 0:1], -1e29), [], [d_["smt"].res])
                for k in range(NIT):
                    last = (k == NIT - 1)
                    for d_ in act_:
                        t_ = d_["smt"].t
                        n_ = d_["NKC"]
                        op("dve", lambda e: e.tensor_scalar(out=d_["nm"].t[:, 0:n_], in0=d_["acc"].t[:, 0:n_], scalar1=t_[:, 0:1], scalar2=None,
                                                            op0=ALU.is_ge, op1=ALU.add, accum_out=t_[:, 3:4]), [d_["acc"].res, d_["smt"].res],
                           [d_["nm"].res, d_["smt"].res])
                    for d_ in act_:
                        t_ = d_["smt"].t
                        op("dve", lambda e: e.tensor_scalar(out=t_[:, 4:5], in0=t_[:, 3:4], scalar1=TOPK - 0.5, scalar2=(-1.0 if last else -0.5),
                                                            op0=ALU.is_ge, op1=ALU.add), [d_["smt"].res], [d_["smt"].res])
                    for d_ in act_:
                        t_ = d_["smt"].t
                        op("dve", lambda e: e.scalar_tensor_tensor(out=t_[:, 0:1], in0=t_[:, 4:5], scalar=t_[:, 8 + k:9 + k], in1=t_[:, 0:1],
                                                                   op0=ALU.mult, op1=ALU.add), [d_["smt"].res], [d_["smt"].res])
                    if k == NIT // 2 and inject is not None and act_:
                        inject()
                        inject = None
                if inject is not None:
                    inject()
                for d_ in st:
                    n_ = d_["NKC"]
                    op("dve", lambda e: e.tensor_scalar(out=d_["nm"].t[:, 0:n_], in0=d_["acc"].t[:, 0:n_], scalar1=d_["smt"].t[:, 0:1], scalar2=-1.0,
                                                        op0=ALU.is_ge, op1=ALU.add), [d_["acc"].res, d_["smt"].res], [d_["nm"].res])
                    ctx[d_["qt"]]["nm"] = d_["nm"]

            def attn_pe(qt):
                gi = gi0 + qt
                nk = gi + 1
                qc = slice(qt * 128, (qt + 1) * 128)
                nm = ctx[qt]["nm"]
                nkb = (nk + 3) // 4
                groups = [(h, jb) for h in range(8) for jb in range(nkb)]

                def logits(h, jb):
                    js = list(range(jb * 4, min(nk, jb * 4 + 4)))
                    lg = PL.next()
                    for jj, j in enumerate(js):
                        op("pe", lambda e: e.matmul(lg.t[:, jj * 128:(jj + 1) * 128], kd[:, j * 128:(j + 1) * 128], qz.t[:, h, qc], start=True, stop=False),
                           [qz_r[h]] + kr, [lg.res])
                        op("pe", lambda e: e.matmul(lg.t[:, jj * 128:(jj + 1) * 128], nm.t[:, j * 128:(j + 1) * 128], bigI, start=False, stop=True),
                           [nm.res, cb.res], [lg.res])
                    pt = PT.next()
                    n = len(js) * 128
                    op("act", lambda e: e.activation(out=pt.t[:, 0:n], in_=lg.t[:, 0:n], func=AF.Exp, scale=0.125), [lg.res], [pt.res])
                    return pt, js

                def pv_acc(h, jb, pt, js):
                    bank = PO[h // 4]
                    hh = h % 4
                    for jj, j in enumerate(js):
                        op("pe", lambda e: e.matmul(bank.t[:, hh * 65:hh * 65 + 65], pt.t[:, jj * 128:(jj + 1) * 128], vaug[li][:, j, :],
                                                    start=(j == 0), stop=(j == nk - 1)), [pt.res] + vr, [bank.res])

                pend = []
                for (h, jb) in groups:
                    pend.append((h, jb) + logits(h, jb))
                    if len(pend) > 1:
                        pv_acc(*pend.pop(0))
                while pend:
                    pv_acc(*pend.pop(0))

            def normalize(qt):
                qc = slice(qt * 128, (qt + 1) * 128)
                for b in range(2):
                    ov = PO[b].t[:, 0:260].rearrange("p (h d) -> p h d", d=65)
                    op("dve", lambda e: e.reciprocal(out=rinv.t[:, 4 * b:4 * b + 4], in_=ov[:, :, 64]), [PO[b].res], [rinv.res])
                    for hh in range(4):
                        h = 4 * b + hh
                        op("dve", lambda e: e.tensor_scalar(out=atok.t[:, h, :], in0=ov[:, hh, 0:64], scalar1=rinv.t[:, h:h + 1], scalar2=None,
                                                            op0=ALU.mult), [PO[b].res, rinv.res], [atok.res])
                ptr = PL.next()
                pvw = ptr.t[:, :].bitcast(BF16)
                for c in range(4):
                    op("pe", lambda e: e.transpose(pvw[:, c * 128:(c + 1) * 128], atok.t[:, 2 * c:2 * c + 2, :].rearrange("p a b -> p (a b)"), identB),
                       [atok.res, cb.res], [ptr.res])
                op("dve", lambda e: e.tensor_tensor(out=yT[:, 0:4, qc], in0=pvw[:, 0:512].rearrange("p (a b) -> p a b", a=4), in1=sgA[:, 0:4, qc],
                                                    op=ALU.mult), [ptr.res] + sgA_r, yT_r[0:4])

            scores(0)
            scores(1)
            bisect_multi([0, 1], None)
            conv_module()
            scores(2)
            scores(3)
            attn_pe(0)
            bisect_multi([2, 3], lambda: normalize(0))
            attn_pe(1)
            normalize(1)
            attn_pe(2)
            normalize(2)
            attn_pe(3)
            normalize(3)
            if STG <= 5:
                return
            out_proj(base + 27)

        def odd_layer(l, blk):
            li = l // 2
            base = LBASE[l]
            norm_to_hT(l)
            for m in range(8):
                dg = DG3.next()
                for j in range(3):
                    wcol = P_CCW + (li * 8 + m) * 3 + j
                    op("pool", lambda e: e.tensor_scalar(out=dg.t[:, j, :], in0=identB, scalar1=par.t[:, wcol:wcol + 1], scalar2=0.0,
                                                         op0=ALU.mult, op1=ALU.add), [cb.res, par.res], [dg.res])
                ps_cg, ps_x, ps_g, ps_b = proj([base + 4 * m + i for i in range(4)], hT, hT_r)
                z = ZB.next()
                if blk == 0:
                    op("pool", lambda e: e.memset(z.t[:, 0:2], 0.0), [], [z.res])
                else:
                    op("pool", lambda e: e.tensor_copy(out=z.t[:, 0:2], in_=zhal.t[:, li, m, :]), [zhal.res], [z.res])
                cg = T32.next()
                op("act", lambda e: e.activation(out=cg.t[:, :], in_=ps_cg.t[:, :], func=AF.Copy), [ps_cg.res], [cg.res])
                op("dve", lambda e: e.tensor_tensor(out=z.t[:, 2:2 + TB], in0=ps_x.t[:, :], in1=cg.t[:, :], op=ALU.mult), [ps_x.res, cg.res], [z.res])
                op("pool", lambda e: e.tensor_copy(out=zhal.t[:, li, m, :], in_=z.t[:, TB:TB + 2]), [z.res], [zhal.res])
                pcv = PS.next()
                for j in range(3):
                    op("pe", lambda e: e.matmul(pcv.t[:, :], dg.t[:, j, :], z.t[:, j:j + TB], start=(j == 0), stop=(j == 2)), [dg.res, z.res], [pcv.res])
                sg = T32.next()
                op("act", lambda e: e.activation(out=sg.t[:, :], in_=ps_g.t[:, :], func=AF.Silu), [ps_g.res], [sg.res])
                t = T32.next()
                op("dve", lambda e: e.tensor_tensor(out=t.t[:, :], in0=pcv.t[:, :], in1=sg.t[:, :], op=ALU.mult), [pcv.res, sg.res], [t.res])
                op("dve", lambda e: e.tensor_tensor(out=yT[:, m, :], in0=ps_b.t[:, :], in1=t.t[:, :], op=ALU.mult), [ps_b.res, t.res], [yT_r[m]])
            out_proj(base + 32)

        def final_out(s, tok0):
            norm_stats()
            for kc in range(8):
                g = par.t[:, P_GF + kc: P_GF + kc + 1]
                op("dve", lambda e: e.scalar_tensor_tensor(out=xT[:, kc, :], in0=xT[:, kc, :], scalar=g, in1=rstd.t[:, :],
                                                           op0=ALU.mult, op1=ALU.mult), [xT_r[kc], rstd.res, par.res], [xT_r[kc]])
            for tt in range(4):
                og = XIN.next()
                for half in range(2):
                    ps = PS.next()
                    for j in range(4):
                        kc = half * 4 + j
                        op("pe", lambda e: e.transpose(ps.t[:, j * 128:(j + 1) * 128], xT[:, kc, tt * 128:(tt + 1) * 128], identF),
                           [xT_r[kc], cst.res], [ps.res])
                    op("act", lambda e: e.activation(out=og.t[:, half * 512:(half + 1) * 512], in_=ps.t[:, :], func=AF.Copy), [ps.res], [og.res])
                S.dma("act", out_d[s, tok0 + tt * 128: tok0 + (tt + 1) * 128, :], og.t[:, :], [og.res], [])

        for s in range(NS):
            for blk in range(NBLK):
                tok0 = blk * TB
                load_x(s, tok0)
                rope_tables(s, tok0)
                if s == 0 and blk == 0:
                    for _ in range(min(n_cast, 16)):
                        prepass_one()
                    w_load_more()
                for l in range(NL):
                    if l % 2 == 0:
                        even_layer(l, blk)
                    else:
                        odd_layer(l, blk)
                final_out(s, tok0)
        S.finish()
        print(f"[build] instructions={S.n_ins} waits={S.n_wait} dmas={S.dma_i} per-engine=" + str({n: e.cnt for n, e in S.engs.items()}), flush=True)
    return nc


def _chunk(cols):
    return np.ascontiguousarray(cols.reshape(8, 128, 128).transpose(1, 0, 2)).reshape(128, 1024)


def prep_weights(w_in_even, w_out_even, w_in_odd, w_out_odd):
    wall = np.zeros((NCH, 128, 1024), np.float32)
    z64 = np.zeros((1024, 56), np.float32)
    for i in range(2):
        We = np.asarray(w_in_even[i], np.float32)
        q, k, v = We[:, 0:512], We[:, 512:576], We[:, 576:640]
        qi, ki, wi = We[:, 640:1152], We[:, 1152:1216], We[:, 1216:1224]
        ga, lin, gg, gb = We[:, 1224:1736], We[:, 1736:2248], We[:, 2248:2760], We[:, 2760:3272]
        ch = []
        for c in range(4):
            ch.append(lin[:, c * 128:(c + 1) * 128])
            ch.append(gg[:, c * 128:(c + 1) * 128])
        for c in range(4):
            ch.append(gb[:, c * 128:(c + 1) * 128])
        for c in range(4):
            ch.append(q[:, c * 128:(c + 1) * 128])
        ch.append(np.concatenate([k, k], axis=1))
        ch.append(np.concatenate([v, wi, z64], axis=1))
        for c in range(4):
            ch.append(qi[:, c * 128:(c + 1) * 128])
        ch.append(np.concatenate([ki, ki], axis=1))
        for c in range(4):
            ch.append(ga[:, c * 128:(c + 1) * 128])
        Wo = np.asarray(w_out_even[i], np.float32)
        for m in range(8):
            ch.append(Wo[:, m * 128:(m + 1) * 128])
        assert len(ch) == NCH_E
        for j, cm in enumerate(ch):
            wall[LBASE[2 * i] + j] = _chunk(cm)
        Wi = np.asarray(w_in_odd[i], np.float32)
        bg, cg, xi, gt = Wi[:, 0:1024], Wi[:, 1024:2048], Wi[:, 2048:3072], Wi[:, 3072:4096]
        ch = []
        for m in range(8):
            sl = slice(m * 128, (m + 1) * 128)
            ch += [cg[:, sl], xi[:, sl], gt[:, sl], bg[:, sl]]
        Wo = np.asarray(w_out_odd[i], np.float32)
        for m in range(8):
            ch.append(Wo[:, m * 128:(m + 1) * 128])
        assert len(ch) == NCH_O
        for j, cm in enumerate(ch):
            wall[LBASE[2 * i + 1] + j] = _chunk(cm)
    return wall


def prep_params(norm_g, final_g, conv_b_w, conv_b_bias, conv_ln_g, conv_ln_b, conv_c_w):
    par = np.zeros((128, NPAR), np.float32)
    par[:, P_G0:P_G0 + 32] = np.asarray(norm_g, np.float32).reshape(4, 8, 128).transpose(2, 0, 1).reshape(128, 32)
    par[:, P_GF:P_GF + 8] = np.asarray(final_g, np.float32).reshape(8, 128).T
    par[:, P_CBW:P_CBW + 248] = np.asarray(conv_b_w, np.float32).reshape(2, 31, 4, 128).transpose(3, 0, 2, 1).reshape(128, 248)
    par[:, P_CBB:P_CBB + 8] = np.asarray(conv_b_bias, np.float32).reshape(2, 4, 128).transpose(2, 0, 1).reshape(128, 8)
    par[:, P_LNG:P_LNG + 8] = np.asarray(conv_ln_g, np.float32).reshape(2, 4, 128).transpose(2, 0, 1).reshape(128, 8)
    par[:, P_LNB:P_LNB + 8] = np.asarray(conv_ln_b, np.float32).reshape(2, 4, 128).transpose(2, 0, 1).reshape(128, 8)
    par[:, P_CCW:P_CCW + 48] = np.asarray(conv_c_w, np.float32).reshape(2, 3, 8, 128).transpose(3, 0, 2, 1).reshape(128, 48)
    half = 32
    inv = (np.float32(10000.0) ** (-(np.arange(half, dtype=np.float32)) / np.float32(half))).astype(np.float32)
    par[:, P_INV] = inv[np.arange(128) % 32]
    par[:, P_EPS] = 1e-6
    par[:, P_CK:P_CK + NIT] = (0.5 ** (np.arange(NIT, dtype=np.float64) + 1)).astype(np.float32)[None, :]
    return par


def prep_consts():
    cst = np.zeros((128, 384), np.float32)
    cst[:, 0:128] = np.eye(128, dtype=np.float32)
    Rm = np.zeros((128, 128), np.float32)
    for hb in (0, 64):
        for d2 in range(64):
            if d2 < 32:
                Rm[hb + d2 + 32, hb + d2] = -1.0
            else:
                Rm[hb + d2 - 32, hb + d2] = 1.0
    cst[:, 128:256] = Rm
    q = np.arange(128)[:, None]
    s = np.arange(128)[None, :]
    cst[:, 256:384] = np.where(s <= q, 0.0, -1e30).astype(np.float32)
    return cst


_CACHE = {}
N_LAUNCH = 1


def run_cores(x, positions, wall, par, cst, n_cores, NS, NL):
    key = (NS, NL)
    if key not in _CACHE:
        _CACHE[key] = build_program(NS, NL)
    nc = _CACHE[key]
    in_maps = []
    for c in range(n_cores):
        in_maps.append({"x": np.ascontiguousarray(x[c * NS:(c + 1) * NS]),
                        "pos": np.ascontiguousarray(positions[c * NS:(c + 1) * NS]).astype(np.int32),
                        "wall": wall, "par": par, "cst": cst})
    res = run_bass_kernel_spmd(nc, in_maps, core_ids=list(range(n_cores)))
    return np.concatenate([r["out"] for r in res.results], axis=0)


def kernel(x, positions, norm_g, w_in_even, w_out_even, conv_b_w, conv_b_bias,
           conv_ln_g, conv_ln_b, w_in_odd, conv_c_w, w_out_odd, final_g):
    x = np.asarray(x, np.float32)
    positions = np.asarray(positions)
    wall = prep_weights(w_in_even, w_out_even, w_in_odd, w_out_odd)
    par = prep_params(norm_g, final_g, conv_b_w, conv_b_bias, conv_ln_g, conv_ln_b, conv_c_w)
    cst = prep_consts()
    n_cores = 8
    n_launch = N_LAUNCH
    NS = x.shape[0] // (n_cores * n_launch)
    outs = []
    per = n_cores * NS
    for i in range(n_launch):
        outs.append(run_cores(x[i * per:(i + 1) * per], positions[i * per:(i + 1) * per], wall, par, cst, n_cores, NS, 4))
    return np.concatenate(outs, axis=0).astype(np.float32)
```

```python
import numpy as np
import concourse.bass as bass
import concourse.mybir as mybir
from concourse.bass_utils import run_bass_kernel_spmd
from contextlib import ExitStack

dt = mybir.dt
F32, BF16, I32 = dt.float32, dt.bfloat16, dt.int32
AF = mybir.ActivationFunctionType
ALU = mybir.AluOpType
AX = mybir.AxisListType

import os as _os
SEM_LIMIT = int(_os.environ.get("MK_SEMLIM", 1000000))


class Res:
    __slots__ = ("name", "w", "r", "excl")

    def __init__(self, name, excl=False):
        self.name = name
        self.w = None
        self.r = {}
        self.excl = excl


class _Eng:
    def __init__(self, name, h, sems):
        self.name = name
        self.h = h
        self.sems = sems
        self.si = 0
        self.cnt = 0
        self.seen = {}
        self.last = None


class Sched:
    def __init__(self, nc, stack, n_eng_sems=14, n_dma_sems=24):
        self.nc = nc
        self.engs = {}
        hs = {"pe": nc.tensor, "act": nc.scalar, "dve": nc.vector, "pool": nc.gpsimd, "sp": nc.sync}
        for n, h in hs.items():
            sems = [stack.enter_context(nc.semaphore(f"s_{n}_{i}")) for i in range(n_eng_sems if n != "sp" else 1)]
            self.engs[n] = _Eng(n, h, sems)
        self.dsems = [stack.enter_context(nc.semaphore(f"s_dma_{i}")) for i in range(n_dma_sems)]
        self.dma_i = 0
        self.dma_tokens = []
        self.n_wait = 0
        self.n_ins = 0

    def res(self, name):
        return Res(name)

    def _wait(self, E, tok):
        sem, val, en = tok
        if E.seen.get(id(sem), 0) >= val:
            return
        E.h.wait_ge(sem, val)
        E.seen[id(sem)] = val
        self.n_wait += 1

    def _deps(self, E, en, reads, writes):
        for r in reads:
            if r.w is not None:
                self._wait(E, r.w)
            if r.excl:
                for k, t in r.r.items():
                    if k != en:
                        self._wait(E, t)
        for w in writes:
            if w.w is not None and not (en == "pe" and w.w[2] == "pe"):
                self._wait(E, w.w)
            for t in w.r.values():
                self._wait(E, t)

    def _record(self, tok, key, reads, writes):
        for r in reads:
            r.r[key] = tok
        for w in writes:
            w.w = tok
            w.r = {}

    def op(self, en, fn, reads=(), writes=()):
        E = self.engs[en]
        self._deps(E, en, reads, writes)
        ins = fn(E.h)
        if E.cnt >= SEM_LIMIT:
            E.si += 1
            E.cnt = 0
        sem = E.sems[E.si]
        E.cnt += 1
        ins.then_inc(sem, 1)
        tok = (sem, E.cnt, en)
        E.last = tok
        self.n_ins += 1
        self._record(tok, en, reads, writes)
        return tok

    def dma(self, qn, out, in_, reads=(), writes=()):
        E = self.engs[qn]
        self._deps(E, qn, reads, writes)
        i = self.dma_i
        self.dma_i += 1
        nd = len(self.dsems)
        sem = self.dsems[i % nd]
        val = 16 * (i // nd + 1)
        if i >= nd:
            self._wait(E, (sem, val - 16, "dma"))
        ins = E.h.dma_start(out=out, in_=in_)
        ins.then_inc(sem, 16)
        tok = (sem, val, "dma")
        self.dma_tokens.append(tok)
        self.n_ins += 1
        self._record(tok, ("dma", i), reads, writes)
        return tok

    def finish(self):
        E = self.engs["sp"]
        for tok in self.dma_tokens[-len(self.dsems):]:
            self._wait(E, tok)
        for n, e in self.engs.items():
            if e.last is not None:
                self._wait(E, e.last)


D = 1024
S_LEN = 2048
TB = 512
NBLK = S_LEN // TB
NCH_E = 35
NCH_O = 40
LBASE = [0, 35, 75, 110]
NCH = 150
NW = 6
NIT = 16
TOPK = 256
P_G0, P_GF, P_CBW, P_CBB, P_LNG, P_LNB, P_CCW, P_INV, P_EPS, P_CK = 0, 32, 40, 288, 296, 304, 312, 360, 361, 368
NPAR = 400
TWO_PI = 6.283185307179586
PI = 3.141592653589793


class _Tile:
    __slots__ = ("t", "res")

    def __init__(self, t, res):
        self.t = t
        self.res = res


class _Rot:
    def __init__(self, tiles):
        self.tiles = tiles
        self.i = 0

    def next(self):
        t = self.tiles[self.i % len(self.tiles)]
        self.i += 1
        return t


def build_program(NS, NL):
    nc = bass.Bass("TRN2", target_bir_lowering=False)
    x_d = nc.dram_tensor("x", [NS, S_LEN, D], F32, kind="ExternalInput").ap()
    pos_d = nc.dram_tensor("pos", [NS, S_LEN], I32, kind="ExternalInput").ap()
    wall_d = nc.dram_tensor("wall", [NCH, 128, 1024], F32, kind="ExternalInput").ap()
    par_d = nc.dram_tensor("par", [128, NPAR], F32, kind="ExternalInput").ap()
    cst_d = nc.dram_tensor("cst", [128, 384], F32, kind="ExternalInput").ap()
    out_d = nc.dram_tensor("out", [NS, S_LEN, D], F32, kind="ExternalOutput").ap()
    wbf_d = nc.dram_tensor("wbf", [NCH, 128, 1024], BF16, kind="Internal").ap()
    dgd_d = nc.dram_tensor("dgd", [8, 128, 31 * 128], BF16, kind="Internal").ap()

    with ExitStack() as st:
        S = Sched(nc, st)
        R = S.res

        def sb(name, shape, dtype):
            return nc.alloc_sbuf_tensor("sb_" + name, shape, dtype)

        def mk(name, shape, dtype):
            return _Tile(sb(name, shape, dtype), R(name))

        def mkrot(name, n, shape, dtype):
            return _Rot([mk(f"{name}{i}", shape, dtype) for i in range(n)])

        par = mk("par", [128, NPAR], F32)
        cst = mk("cst", [128, 384], F32)
        identF = cst.t[:, 0:128]
        causneg = cst.t[:, 256:384]
        cb = mk("cb", [128, 4, 128], BF16)
        identB, onesB, bigI, RmB = cb.t[:, 0, :], cb.t[:, 1, :], cb.t[:, 2, :], cb.t[:, 3, :]
        xT = sb("xT", [128, 8, TB], F32)
        xT_r = [R(f"xT{k}") for k in range(8)]
        hT = sb("hT", [128, 8, TB], BF16)
        hT_r = [R(f"hT{k}") for k in range(8)]
        yT = sb("yT", [128, 8, TB], BF16)
        yT_r = [R(f"yT{k}") for k in range(8)]
        rstd = mk("rstd", [128, TB], F32)
        cosT = mk("cosT", [128, TB], F32)
        sinT = mk("sinT", [128, TB], F32)
        kdup = [sb(f"kdup{i}", [128, S_LEN], BF16) for i in range(2)]
        kidup = [sb(f"kidup{i}", [128, S_LEN], BF16) for i in range(2)]
        kdup_r = [[R(f"kdup{i}_{b}") for b in range(NBLK)] for i in range(2)]
        kidup_r = [[R(f"kidup{i}_{b}") for b in range(NBLK)] for i in range(2)]
        vaug = [sb(f"vaug{i}", [128, 16, 65], BF16) for i in range(2)]
        vaug_r = [[R(f"vaug{i}_{b}") for b in range(NBLK)] for i in range(2)]
        qz = mk("qz", [128, 8, TB], BF16)
        qiz = mk("qiz", [128, 8, TB], BF16)
        qz_r = [R(f"qz{h}") for h in range(8)]
        qiz_r = [R(f"qiz{h}") for h in range(8)]
        sgA = sb("sgA", [128, 4, TB], BF16)
        sgA_r = [R(f"sgA{c}") for c in range(4)]
        sgB = sb("sgB", [128, 4, TB], BF16)
        sgB_r = [R(f"sgB{c}") for c in range(4)]
        cglu = sb("cglu", [128, 4, 30 + TB], BF16)
        cglu_r = [R(f"cglu{c}") for c in range(4)]
        chal = mk("chal", [128, 2, 4, 30], BF16)
        zhal = mk("zhal", [128, 2, 8, 2], BF16)
        xc = sb("xc", [128, 4, TB], BF16)
        xc_r = [R(f"xc{c}") for c in range(4)]
        witok = mk("witok", [128, 4, 8], F32)
        wring = mkrot("wr", NW, [128, 8, 128], BF16)
        DG = mkrot("dg", 2, [128, 31, 128], BF16)
        DG3 = mkrot("dg3", 2, [128, 3, 128], BF16)
        DW = mkrot("dgw", 2, [128, 8, 128], BF16)
        ZB = mkrot("zb", 2, [128, 2 + TB], BF16)
        ACC = mkrot("acc", 2, [128, S_LEN], F32)
        NM = mkrot("nm", 4, [128, S_LEN], BF16)
        R16 = mkrot("r16", 3, [128, 512], BF16)
        PT = mkrot("pt", 3, [128, 512], BF16)
        T32 = mkrot("t32", 8, [128, 512], F32)
        T16 = mkrot("t16", 4, [128, 512], BF16)
        XIN = mkrot("xin", 2, [128, 1024], F32)
        WST = mkrot("wst", 2, [128, 1024], BF16)
        atok = mk("atok", [128, 8, 64], BF16)
        SM = mkrot("sm", 2, [128, 32], F32)
        rinv = mk("rinv", [128, 8], F32)
        posi = mk("posi", [128, TB], I32)
        RT = T32
        RTI = mk("rti", [128, TB], I32)

        banks = [_Tile(nc.alloc_psum_tensor(f"ps{i}", [128, 512], F32), Res(f"ps{i}", excl=True)) for i in range(8)]
        PS = _Rot(banks)
        PA = _Rot(banks[0:2])
        PD = _Rot(banks[2:4])
        PO = banks[4:6]
        PL = _Rot(banks[6:8])

        wbf_r = [R(f"wbf{c}") for c in range(NCH)]
        dgd_r = [R(f"dgd{c}") for c in range(8)]

        def op(en, fn, reads=(), writes=()):
            return S.op(en, fn, list(reads), list(writes))

        S.dma("sp", par.t[:, :], par_d, [], [par.res])
        S.dma("sp", cst.t[:, :], cst_d, [], [cst.res])
        n_cast = LBASE[NL] if NL < 4 else NCH
        cast_engs = ["act", "dve", "pool"]
        pp = {"next": 0}

        def prepass_one():
            c = pp["next"]
            if c >= n_cast:
                return
            pp["next"] += 1
            sf = XIN.next()
            sbf = WST.next()
            S.dma("sp", sf.t[:, :], wall_d[c], [], [sf.res])
            en = cast_engs[c % 3]
            if en == "act":
                op("act", lambda e: e.activation(out=sbf.t[:, :], in_=sf.t[:, :], func=AF.Copy), [sf.res], [sbf.res])
            else:
                op(en, lambda e: e.tensor_copy(out=sbf.t[:, :], in_=sf.t[:, :]), [sf.res], [sbf.res])
            S.dma("sp", wbf_d[c], sbf.t[:, :], [sbf.res], [wbf_r[c]])

        op("dve", lambda e: e.tensor_copy(out=identB, in_=identF), [cst.res], [cb.res])
        op("dve", lambda e: e.memset(onesB, 1.0), [], [cb.res])
        op("dve", lambda e: e.tensor_scalar(out=bigI, in0=identF, scalar1=32768.0, scalar2=None, op0=ALU.mult), [cst.res], [cb.res])
        op("dve", lambda e: e.tensor_copy(out=RmB, in_=cst.t[:, 128:256]), [cst.res], [cb.res])
        op("dve", lambda e: e.memset(qz.t[:, :, :], 0.0), [], [qz.res] + qz_r)
        op("dve", lambda e: e.memset(qiz.t[:, :, :], 0.0), [], [qiz.res] + qiz_r)
        for i in range(2):
            op("dve", lambda e: e.memset(vaug[i][:, :, :], 1.0), [], vaug_r[i])

        for i8 in range(8):
            dg = DG.next()
            for j in range(31):
                wcol = P_CBW + i8 * 31 + j
                op("pool", lambda e: e.tensor_scalar(out=dg.t[:, j, :], in0=identB, scalar1=par.t[:, wcol:wcol + 1], scalar2=0.0,
                                                     op0=ALU.mult, op1=ALU.add), [cb.res, par.res], [dg.res])
            S.dma("sp", dgd_d[i8], dg.t[:, :, :].rearrange("p a b -> p (a b)"), [dg.res], [dgd_r[i8]])

        import os
        STG = float(os.environ.get("MK_STAGE", 99))
        per_blk = LBASE[NL] if NL < 4 else NCH
        if STG < 6:
            per_blk = {1.0: 12, 1.2: 16, 1.4: 17, 1.6: 18}.get(STG, 27)
        total_w = NS * NBLK * per_blk
        ws = {"load": 0, "use": 0}

        def w_load_more():
            while ws["load"] < total_w and ws["load"] < ws["use"] + NW:
                i = ws["load"]
                ch = i % per_blk
                assert wbf_r[ch].w is not None, ch
                slot = wring.tiles[i % NW]
                S.dma("sp", slot.t[:, :, :], wbf_d[ch].rearrange("p (k m) -> p k m", k=8), [wbf_r[ch]], [slot.res])
                ws["load"] += 1

        def w_use(ch):
            i = ws["use"]
            assert i % per_blk == ch, (i, per_blk, ch)
            ws["use"] += 1
            return wring.tiles[i % NW]

        def proj(chs, rhs, rhs_r):
            outs = []
            for ch in chs:
                w = w_use(ch)
                ps = PS.next()
                for kc in range(8):
                    op("pe", lambda e: e.matmul(ps.t[:, :], w.t[:, kc, :], rhs[:, kc, :], start=(kc == 0), stop=(kc == 7)),
                       [w.res, rhs_r[kc]], [ps.res])
                w_load_more()
                prepass_one()
                outs.append(ps)
            return outs

        def load_x(s, tok0):
            for tt in range(4):
                xin = XIN.next()
                S.dma("sp", xin.t[:, :], x_d[s, tok0 + tt * 128: tok0 + (tt + 1) * 128, :], [], [xin.res])
                for half in range(2):
                    ps = PS.next()
                    for j in range(4):
                        kc = half * 4 + j
                        op("pe", lambda e: e.transpose(ps.t[:, j * 128:(j + 1) * 128], xin.t[:, kc * 128:(kc + 1) * 128], identF),
                           [xin.res, cst.res], [ps.res])
                    op("act", lambda e: e.activation(out=xT[:, half * 4:half * 4 + 4, tt * 128:(tt + 1) * 128],
                                                     in_=ps.t[:, :].rearrange("p (a b) -> p a b", a=4), func=AF.Copy),
                       [ps.res], xT_r[half * 4:half * 4 + 4])

        def rope_tables(s, tok0):
            S.dma("sp", posi.t[:, :], pos_d[s:s + 1, tok0:tok0 + TB].partition_broadcast(128), [], [posi.res])
            posf = RT.next()
            op("pool", lambda e: e.tensor_copy(out=posf.t[:, :], in_=posi.t[:, :]), [posi.res], [posf.res])
            ang = RT.next()
            op("pool", lambda e: e.tensor_scalar(out=ang.t[:, :], in0=posf.t[:, :], scalar1=par.t[:, P_INV:P_INV + 1], scalar2=0.0,
                                                 op0=ALU.mult, op1=ALU.add), [posf.res, par.res], [ang.res])
            for dst, shift in ((sinT, 0.0), (cosT, PI / 2)):
                t = RT.next()
                op("pool", lambda e: e.tensor_scalar(out=t.t[:, :], in0=ang.t[:, :], scalar1=1.0 / TWO_PI, scalar2=shift / TWO_PI,
                                                     op0=ALU.mult, op1=ALU.add), [ang.res], [t.res])
                op("pool", lambda e: e.tensor_copy(out=RTI.t[:, :], in_=t.t[:, :]), [t.res], [RTI.res])
                op("pool", lambda e: e.tensor_copy(out=t.t[:, :], in_=RTI.t[:, :]), [RTI.res], [t.res])
                r = RT.next()
                op("dve", lambda e: e.scalar_tensor_tensor(out=r.t[:, :], in0=t.t[:, :], scalar=-TWO_PI, in1=ang.t[:, :],
                                                           op0=ALU.mult, op1=ALU.add), [t.res, ang.res], [r.res])
                if shift != 0.0:
                    op("dve", lambda e: e.tensor_scalar(out=r.t[:, :], in0=r.t[:, :], scalar1=shift, scalar2=None, op0=ALU.add),
                       [r.res], [r.res])
                op("dve", lambda e: e.tensor_scalar(out=t.t[:, :], in0=r.t[:, :], scalar1=PI, scalar2=-TWO_PI, op0=ALU.is_gt, op1=ALU.mult),
                   [r.res], [t.res])
                op("dve", lambda e: e.tensor_tensor(out=r.t[:, :], in0=r.t[:, :], in1=t.t[:, :], op=ALU.add), [r.res, t.res], [r.res])
                op("dve", lambda e: e.tensor_scalar(out=t.t[:, :], in0=r.t[:, :], scalar1=-PI, scalar2=TWO_PI, op0=ALU.is_lt, op1=ALU.mult),
                   [r.res], [t.res])
                op("dve", lambda e: e.tensor_tensor(out=r.t[:, :], in0=r.t[:, :], in1=t.t[:, :], op=ALU.add), [r.res, t.res], [r.res])
                op("dve", lambda e: e.tensor_scalar(out=r.t[:, :], in0=r.t[:, :], scalar1=3.1415925, scalar2=-3.1415925, op0=ALU.min, op1=ALU.max),
                   [r.res], [r.res])
                op("act", lambda e: e.activation(out=dst.t[:, :], in_=r.t[:, :], func=AF.Sin), [r.res], [dst.res])

        def norm_stats():
            ps = PS.next()
            for kc in range(8):
                sq = T16.next()
                op("act", lambda e: e.activation(out=sq.t[:, :], in_=xT[:, kc, :], func=AF.Square), [xT_r[kc]], [sq.res])
                op("pe", lambda e: e.matmul(ps.t[:, :], onesB, sq.t[:, :], start=(kc == 0), stop=(kc == 7)), [sq.res, cb.res], [ps.res])
            sd = T32.next()
            op("act", lambda e: e.activation(out=sd.t[:, :], in_=ps.t[:, :], func=AF.Sqrt, scale=1.0 / D, bias=par.t[:, P_EPS:P_EPS + 1]),
               [ps.res, par.res], [sd.res])
            op("dve", lambda e: e.reciprocal(out=rstd.t[:, :], in_=sd.t[:, :]), [sd.res], [rstd.res])

        def norm_to_hT(l):
            norm_stats()
            for kc in range(8):
                g = par.t[:, P_G0 + l * 8 + kc: P_G0 + l * 8 + kc + 1]
                op("dve", lambda e: e.scalar_tensor_tensor(out=hT[:, kc, :], in0=xT[:, kc, :], scalar=g, in1=rstd.t[:, :],
                                                           op0=ALU.mult, op1=ALU.mult), [xT_r[kc], rstd.res, par.res], [hT_r[kc]])

        def rope(ps, dests):
            u1 = T16.next()
            op("dve", lambda e: e.tensor_tensor(out=u1.t[:, :], in0=ps.t[:, :], in1=cosT.t[:, :], op=ALU.mult), [ps.res, cosT.res], [u1.res])
            u2 = T16.next()
            op("dve", lambda e: e.tensor_tensor(out=u2.t[:, :], in0=ps.t[:, :], in1=sinT.t[:, :], op=ALU.mult), [ps.res, sinT.res], [u2.res])
            ps2 = PS.next()
            op("pe", lambda e: e.matmul(ps2.t[:, :], identB, u1.t[:, :], start=True, stop=False), [u1.res, cb.res], [ps2.res])
            op("pe", lambda e: e.matmul(ps2.t[:, :], RmB, u2.t[:, :], start=False, stop=True), [u2.res, cb.res], [ps2.res])
            for rows, out_ap, res in dests:
                op("act", lambda e: e.activation(out=out_ap, in_=ps2.t[rows, :], func=AF.Copy), [ps2.res], [res])

        def out_proj(base):
            for m in range(8):
                (ps,) = proj([base + m], yT, yT_r)
                op("dve", lambda e: e.tensor_tensor(out=xT[:, m, :], in0=ps.t[:, :], in1=xT[:, m, :], op=ALU.add), [ps.res, xT_r[m]], [xT_r[m]])

        def even_layer(l, blk):
            li = l // 2
            base = LBASE[l]
            gi0 = blk * 4
            tok0 = blk * TB
            norm_to_hT(l)
            dgs = {}

            def dg_load(cc):
                dg = DG.next()
                S.dma("sp", dg.t[:, :, :].rearrange("p a b -> p (a b)"), dgd_d[li * 4 + cc], [dgd_r[li * 4 + cc]], [dg.res])
                dgs[cc] = dg
            dg_load(0)
            dg_load(1)
            if blk == 0:
                op("pool", lambda e: e.memset(cglu[:, :, 0:30], 0.0), [], cglu_r)
            else:
                op("pool", lambda e: e.tensor_copy(out=cglu[:, :, 0:30], in_=chal.t[:, li, :, :]), [chal.res], cglu_r)
            for c in range(4):
                ps_l, ps_g = proj([base + 2 * c, base + 2 * c + 1], hT, hT_r)
                sig = T32.next()
                op("act", lambda e: e.activation(out=sig.t[:, :], in_=ps_g.t[:, :], func=AF.Sigmoid), [ps_g.res], [sig.res])
                op("dve", lambda e: e.tensor_tensor(out=cglu[:, c, 30:30 + TB], in0=ps_l.t[:, :], in1=sig.t[:, :], op=ALU.mult),
                   [ps_l.res, sig.res], [cglu_r[c]])
            op("pool", lambda e: e.tensor_copy(out=chal.t[:, li, :, :], in_=cglu[:, :, TB:TB + 30]), cglu_r, [chal.res])
            for c in range(4):
                (ps,) = proj([base + 8 + c], hT, hT_r)
                op("act", lambda e: e.activation(out=sgB[:, c, :], in_=ps.t[:, :], func=AF.Silu), [ps.res], [sgB_r[c]])
            if STG <= 1:
                return
            for c in range(4):
                (ps,) = proj([base + 12 + c], hT, hT_r)

                rope(ps, [(slice(0, 64), qz.t[0:64, 2 * c, :], qz_r[2 * c]), (slice(64, 128), qz.t[64:128, 2 * c + 1, :], qz_r[2 * c + 1])])
            if STG <= 1.2:
                return
            (ps,) = proj([base + 16], hT, hT_r)

            rope(ps, [(slice(0, 128), kdup[li][:, tok0:tok0 + TB], kdup_r[li][blk])])
            if STG <= 1.4:
                return
            (ps,) = proj([base + 17], hT, hT_r)
            vw = T32.next()
            op("act", lambda e: e.activation(out=vw.t[:, :], in_=ps.t[:, :], func=AF.Copy), [ps.res], [vw.res])
            pst = PS.next()
            for tt in range(4):
                op("pe", lambda e: e.transpose(pst.t[:, tt * 128:(tt + 1) * 128], vw.t[:, tt * 128:(tt + 1) * 128], identF),
                   [vw.res, cst.res], [pst.res])
            pv = pst.t[:, :].rearrange("p (a b) -> p a b", a=4)
            op("act", lambda e: e.activation(out=vaug[li][:, gi0:gi0 + 4, 0:64], in_=pv[:, :, 0:64], func=AF.Copy), [pst.res], [vaug_r[li][blk]])
            op("dve", lambda e: e.tensor_scalar(out=witok.t[:, :, :], in0=pv[:, :, 64:72], scalar1=float(8 ** -0.5 * 64 ** -0.5), scalar2=None,
                                                op0=ALU.mult), [pst.res], [witok.res])
            if STG <= 1.6:
                return
            for c in range(4):
                (ps,) = proj([base + 18 + c], hT, hT_r)

                rope(ps, [(slice(0, 64), qiz.t[0:64, 2 * c, :], qiz_r[2 * c]), (slice(64, 128), qiz.t[64:128, 2 * c + 1, :], qiz_r[2 * c + 1])])
            (ps,) = proj([base + 22], hT, hT_r)

            rope(ps, [(slice(0, 128), kidup[li][:, tok0:tok0 + TB], kidup_r[li][blk])])
            for c in range(4):
                (ps,) = proj([base + 23 + c], hT, hT_r)
                op("act", lambda e: e.activation(out=sgA[:, c, :], in_=ps.t[:, :], func=AF.Silu), [ps.res], [sgA_r[c]])

            if STG <= 2:
                return
            def conv_module():
                s1 = PS.next()
                s2 = PS.next()
                for cc in range(4):
                    dg = dgs[cc]
                    ps = PS.next()
                    for j in range(31):
                        op("pe", lambda e: e.matmul(ps.t[:, :], dg.t[:, j, :], cglu[:, cc, j:j + TB], start=(j == 0), stop=(j == 30)),
                           [dg.res, cglu_r[cc]], [ps.res])
                    if cc + 2 < 4:
                        dg_load(cc + 2)
                    bcol = par.t[:, P_CBB + li * 4 + cc: P_CBB + li * 4 + cc + 1]
                    op("act", lambda e: e.activation(out=xc[:, cc, :], in_=ps.t[:, :], func=AF.Identity, bias=bcol, scale=1.0),
                       [ps.res, par.res], [xc_r[cc]])
                    sq = T16.next()
                    op("act", lambda e: e.activation(out=sq.t[:, :], in_=ps.t[:, :], func=AF.Square, bias=bcol, scale=1.0),
                       [ps.res, par.res], [sq.res])
                    op("pe", lambda e: e.matmul(s1.t[:, :], onesB, xc[:, cc, :], start=(cc == 0), stop=(cc == 3)), [xc_r[cc], cb.res], [s1.res])
                    op("pe", lambda e: e.matmul(s2.t[:, :], onesB, sq.t[:, :], start=(cc == 0), stop=(cc == 3)), [sq.res, cb.res], [s2.res])
                mean = T32.next()
                op("dve", lambda e: e.tensor_scalar(out=mean.t[:, :], in0=s1.t[:, :], scalar1=1.0 / 512, scalar2=None, op0=ALU.mult), [s1.res], [mean.res])
                msq = T32.next()
                op("pool", lambda e: e.tensor_tensor(out=msq.t[:, :], in0=mean.t[:, :], in1=mean.t[:, :], op=ALU.mult), [mean.res], [msq.res])
                var = T32.next()
                op("dve", lambda e: e.scalar_tensor_tensor(out=var.t[:, :], in0=s2.t[:, :], scalar=1.0 / 512, in1=msq.t[:, :],
                                                           op0=ALU.mult, op1=ALU.subtract), [s2.res, msq.res], [var.res])
                op("dve", lambda e: e.tensor_scalar(out=var.t[:, :], in0=var.t[:, :], scalar1=0.0, scalar2=None, op0=ALU.max), [var.res], [var.res])
                sd = T32.next()
                op("act", lambda e: e.activation(out=sd.t[:, :], in_=var.t[:, :], func=AF.Sqrt, scale=1.0, bias=par.t[:, P_EPS:P_EPS + 1]),
                   [var.res, par.res], [sd.res])
                rs = T32.next()
                op("dve", lambda e: e.reciprocal(out=rs.t[:, :], in_=sd.t[:, :]), [sd.res], [rs.res])
                for cc in range(4):
                    d = T32.next()
                    op("dve", lambda e: e.tensor_tensor(out=d.t[:, :], in0=xc[:, cc, :], in1=mean.t[:, :], op=ALU.subtract), [xc_r[cc], mean.res], [d.res])
                    op("dve", lambda e: e.tensor_tensor(out=d.t[:, :], in0=d.t[:, :], in1=rs.t[:, :], op=ALU.mult), [d.res, rs.res], [d.res])
                    sl = T16.next()
                    gcol = par.t[:, P_LNG + li * 4 + cc: P_LNG + li * 4 + cc + 1]
                    bcol2 = par.t[:, P_LNB + li * 4 + cc: P_LNB + li * 4 + cc + 1]
                    op("act", lambda e: e.activation(out=sl.t[:, :], in_=d.t[:, :], func=AF.Silu, scale=gcol, bias=bcol2), [d.res, par.res], [sl.res])
                    op("pool", lambda e: e.tensor_tensor(out=yT[:, 4 + cc, :], in0=sl.t[:, :], in1=sgB[:, cc, :], op=ALU.mult),
                       [sl.res, sgB_r[cc]], [yT_r[4 + cc]])


            kd = kdup[li]
            kid = kidup[li]
            junk = xc[:, :, :].rearrange("p a b -> p (a b)")
            krs = kidup_r[li][0:blk + 1]
            kr = kdup_r[li][0:blk + 1]
            vr = vaug_r[li][0:blk + 1]
            ctx = {}

            def scores(qt):
                gi = gi0 + qt
                nk = gi + 1
                NKC = nk * 128
                qc = slice(qt * 128, (qt + 1) * 128)
                acc = ACC.next()
                dgw = DW.next()
                for h in range(8):
                    op("pool", lambda e: e.tensor_scalar(out=dgw.t[:, h, :], in0=identB, scalar1=witok.t[:, qt, h:h + 1], scalar2=0.0,
                                                         op0=ALU.mult, op1=ALU.add), [cb.res, witok.res], [dgw.res])
                nkb = (nk + 3) // 4
                steps = [(kb, h) for kb in range(nkb) for h in range(8)]
                abank = {}
                pend = []

                def dots(kb, h):
                    c0 = kb * 512
                    n = min(NKC, c0 + 512) - c0
                    psd = PD.next()
                    op("pe", lambda e: e.matmul(psd.t[:, 0:n], qiz.t[:, h, qc], kid[:, c0:c0 + n], start=True, stop=True),
                       [qiz_r[h]] + krs, [psd.res])
                    r = R16.next()
                    op("act", lambda e: e.activation(out=r.t[:, 0:n], in_=psd.t[:, 0:n], func=AF.Relu), [psd.res], [r.res])
                    return r, n

                def wsum(kb, h, r, n):
                    if h == 0:
                        abank[kb] = PA.next()
                    a = abank[kb]
                    op("pe", lambda e: e.matmul(a.t[:, 0:n], dgw.t[:, h, :], r.t[:, 0:n], start=(h == 0), stop=(h == 7)), [dgw.res, r.res], [a.res])
                    if h == 7:
                        c0 = kb * 512
                        op("act", lambda e: e.activation(out=acc.t[:, c0:c0 + n], in_=a.t[:, 0:n], func=AF.Copy), [a.res], [acc.res])

                for (kb, h) in steps:
                    pend.append((kb, h) + dots(kb, h))
                    if len(pend) > 2:
                        wsum(*pend.pop(0))
                while pend:
                    wsum(*pend.pop(0))
                ctx[qt] = {"acc": acc, "nm": None}

            def bisect_multi(qts, inject):
                st = []
                for qt in qts:
                    gi = gi0 + qt
                    st.append(dict(qt=qt, gi=gi, NKC=(gi + 1) * 128, acc=ctx[qt]["acc"], smt=SM.next(), nm=NM.next()))
                for d_ in st:
                    if d_["gi"] >= 2:
                        op("dve", lambda e: e.tensor_reduce(out=d_["smt"].t[:, 1:2], in_=d_["acc"].t[:, 0:d_["NKC"]], axis=AX.X, op=ALU.max,
                                                            apply_absolute_value=True), [d_["acc"].res], [d_["smt"].res])
                for d_ in st:
                    g0 = d_["gi"] * 128
                    op("pool", lambda e: e.tensor_tensor(out=d_["acc"].t[:, g0:g0 + 128], in0=d_["acc"].t[:, g0:g0 + 128],
                                                         in1=causneg, op=ALU.add), [d_["acc"].res, cst.res, d_["smt"].res], [d_["acc"].res])
                act_ = [d_ for d_ in st if d_["gi"] >= 2]
                for d_ in st:
                    t_ = d_["smt"].t
                    if d_["gi"] >= 2:
                        op("dve", lambda e: e.tensor_scalar(out=t_[:, 0:1], in0=t_[:, 1:2], scalar1=-1.0, scalar2=None, op0=ALU.mult), [d_["smt"].res], [d_["smt"].res])
                        op("dve", lambda e: e.tensor_scalar(out=t_[:, 2:3], in0=t_[:, 1:2], scalar1=2.0002, scalar2=1e-20, op0=ALU.mult, op1=ALU.add),
                           [d_["smt"].res], [d_["smt"].res])
                        op("dve", lambda e: e.tensor_scalar(out=t_[:, 8:8 + NIT], in0=par.t[:, P_CK:P_CK + NIT], scalar1=t_[:, 2:3], scalar2=None, op0=ALU.mult),
                           [d_["smt"].res, par.res], [d_["smt"].res])
                        op("dve", lambda e: e.tensor_tensor(out=t_[:, 0:1], in0=t_[:, 0:1], in1=t_[:, 8:9], op=ALU.add), [d_["smt"].res], [d_["smt"].res])
                    else:
                        op("dve", lambda e: e.memset(t_[:, 0:1], -1e29), [], [d_["smt"].res])
                for k in range(NIT):
                    last = (k == NIT - 1)
                    for d_ in act_:
                        t_ = d_["smt"].t
                        n_ = d_["NKC"]
                        op("dve", lambda e: e.tensor_scalar(out=d_["nm"].t[:, 0:n_], in0=d_["acc"].t[:, 0:n_], scalar1=t_[:, 0:1], scalar2=None,
                                                            op0=ALU.is_ge, op1=ALU.add, accum_out=t_[:, 3:4]), [d_["acc"].res, d_["smt"].res],
                           [d_["nm"].res, d_["smt"].res])
                    for d_ in act_:
                        t_ = d_["smt"].t
                        op("dve", lambda e: e.tensor_scalar(out=t_[:, 4:5], in0=t_[:, 3:4], scalar1=TOPK - 0.5, scalar2=(-1.0 if last else -0.5),
                                                            op0=ALU.is_ge, op1=ALU.add), [d_["smt"].res], [d_["smt"].res])
                    for d_ in act_:
                        t_ = d_["smt"].t
                        op("dve", lambda e: e.scalar_tensor_tensor(out=t_[:, 0:1], in0=t_[:, 4:5], scalar=t_[:, 8 + k:9 + k], in1=t_[:, 0:1],
                                                                   op0=ALU.mult, op1=ALU.add), [d_["smt"].res], [d_["smt"].res])
                    if k == NIT // 2 and inject is not None and act_:
                        inject()
                        inject = None
                if inject is not None:
                    inject()
                for d_ in st:
                    n_ = d_["NKC"]
                    op("dve", lambda e: e.tensor_scalar(out=d_["nm"].t[:, 0:n_], in0=d_["acc"].t[:, 0:n_], scalar1=d_["smt"].t[:, 0:1], scalar2=-1.0,
                                                        op0=ALU.is_ge, op1=ALU.add), [d_["acc"].res, d_["smt"].res], [d_["nm"].res])
                    ctx[d_["qt"]]["nm"] = d_["nm"]

            def attn_pe(qt):
                gi = gi0 + qt
                nk = gi + 1
                qc = slice(qt * 128, (qt + 1) * 128)
                nm = ctx[qt]["nm"]
                nkb = (nk + 3) // 4
                groups = [(h, jb) for h in range(8) for jb in range(nkb)]

                def logits(h, jb):
                    js = list(range(jb * 4, min(nk, jb * 4 + 4)))
                    lg = PL.next()
                    for jj, j in enumerate(js):
                        op("pe", lambda e: e.matmul(lg.t[:, jj * 128:(jj + 1) * 128], kd[:, j * 128:(j + 1) * 128], qz.t[:, h, qc], start=True, stop=False),
                           [qz_r[h]] + kr, [lg.res])
                        op("pe", lambda e: e.matmul(lg.t[:, jj * 128:(jj + 1) * 128], nm.t[:, j * 128:(j + 1) * 128], bigI, start=False, stop=True),
                           [nm.res, cb.res], [lg.res])
                    pt = PT.next()
                    n = len(js) * 128
                    op("act", lambda e: e.activation(out=pt.t[:, 0:n], in_=lg.t[:, 0:n], func=AF.Exp, scale=0.125), [lg.res], [pt.res])
                    return pt, js

                def pv_acc(h, jb, pt, js):
                    bank = PO[h // 4]
                    hh = h % 4
                    for jj, j in enumerate(js):
                        op("pe", lambda e: e.matmul(bank.t[:, hh * 65:hh * 65 + 65], pt.t[:, jj * 128:(jj + 1) * 128], vaug[li][:, j, :],
                                                    start=(j == 0), stop=(j == nk - 1)), [pt.res] + vr, [bank.res])

                pend = []
                for (h, jb) in groups:
                    pend.append((h, jb) + logits(h, jb))
                    if len(pend) > 2:
                        pv_acc(*pend.pop(0))
                while pend:
                    pv_acc(*pend.pop(0))

            def normalize(qt):
                qc = slice(qt * 128, (qt + 1) * 128)
                for b in range(2):
                    ov = PO[b].t[:, 0:260].rearrange("p (h d) -> p h d", d=65)
                    op("dve", lambda e: e.reciprocal(out=rinv.t[:, 4 * b:4 * b + 4], in_=ov[:, :, 64]), [PO[b].res], [rinv.res])
                    for hh in range(4):
                        h = 4 * b + hh
                        op("dve", lambda e: e.tensor_scalar(out=atok.t[:, h, :], in0=ov[:, hh, 0:64], scalar1=rinv.t[:, h:h + 1], scalar2=None,
                                                            op0=ALU.mult), [PO[b].res, rinv.res], [atok.res])
                ptr = PL.next()
                pvw = ptr.t[:, :].bitcast(BF16)
                for c in range(4):
                    op("pe", lambda e: e.transpose(pvw[:, c * 128:(c + 1) * 128], atok.t[:, 2 * c:2 * c + 2, :].rearrange("p a b -> p (a b)"), identB),
                       [atok.res, cb.res], [ptr.res])
                op("dve", lambda e: e.tensor_tensor(out=yT[:, 0:4, qc], in0=pvw[:, 0:512].rearrange("p (a b) -> p a b", a=4), in1=sgA[:, 0:4, qc],
                                                    op=ALU.mult), [ptr.res] + sgA_r, yT_r[0:4])

            scores(0)
            scores(1)
            bisect_multi([0, 1], None)
            conv_module()
            scores(2)
            scores(3)
            attn_pe(0)
            bisect_multi([2, 3], lambda: normalize(0))
            attn_pe(1)
            normalize(1)
            attn_pe(2)
            normalize(2)
            attn_pe(3)
            normalize(3)
            if STG <= 5:
                return
            out_proj(base + 27)

        def odd_layer(l, blk):
            li = l // 2
            base = LBASE[l]
            norm_to_hT(l)
            for m in range(8):
                dg = DG3.next()
                for j in range(3):
                    wcol = P_CCW + (li * 8 + m) * 3 + j
                    op("pool", lambda e: e.tensor_scalar(out=dg.t[:, j, :], in0=identB, scalar1=par.t[:, wcol:wcol + 1], scalar2=0.0,
                                                         op0=ALU.mult, op1=ALU.add), [cb.res, par.res], [dg.res])
                ps_cg, ps_x, ps_g, ps_b = proj([base + 4 * m + i for i in range(4)], hT, hT_r)
                z = ZB.next()
                if blk == 0:
                    op("pool", lambda e: e.memset(z.t[:, 0:2], 0.0), [], [z.res])
                else:
                    op("pool", lambda e: e.tensor_copy(out=z.t[:, 0:2], in_=zhal.t[:, li, m, :]), [zhal.res], [z.res])
                cg = T32.next()
                op("act", lambda e: e.activation(out=cg.t[:, :], in_=ps_cg.t[:, :], func=AF.Copy), [ps_cg.res], [cg.res])
                op("dve", lambda e: e.tensor_tensor(out=z.t[:, 2:2 + TB], in0=ps_x.t[:, :], in1=cg.t[:, :], op=ALU.mult), [ps_x.res, cg.res], [z.res])
                op("pool", lambda e: e.tensor_copy(out=zhal.t[:, li, m, :], in_=z.t[:, TB:TB + 2]), [z.res], [zhal.res])
                pcv = PS.next()
                for j in range(3):
                    op("pe", lambda e: e.matmul(pcv.t[:, :], dg.t[:, j, :], z.t[:, j:j + TB], start=(j == 0), stop=(j == 2)), [dg.res, z.res], [pcv.res])
                sg = T32.next()
                op("act", lambda e: e.activation(out=sg.t[:, :], in_=ps_g.t[:, :], func=AF.Silu), [ps_g.res], [sg.res])
                t = T32.next()
                op("dve", lambda e: e.tensor_tensor(out=t.t[:, :], in0=pcv.t[:, :], in1=sg.t[:, :], op=ALU.mult), [pcv.res, sg.res], [t.res])
                op("dve", lambda e: e.tensor_tensor(out=yT[:, m, :], in0=ps_b.t[:, :], in1=t.t[:, :], op=ALU.mult), [ps_b.res, t.res], [yT_r[m]])
            out_proj(base + 32)

        def final_out(s, tok0):
            norm_stats()
            for kc in range(8):
                g = par.t[:, P_GF + kc: P_GF + kc + 1]
                op("dve", lambda e: e.scalar_tensor_tensor(out=xT[:, kc, :], in0=xT[:, kc, :], scalar=g, in1=rstd.t[:, :],
                                                           op0=ALU.mult, op1=ALU.mult), [xT_r[kc], rstd.res, par.res], [xT_r[kc]])
            for tt in range(4):
                og = XIN.next()
                for half in range(2):
                    ps = PS.next()
                    for j in range(4):
                        kc = half * 4 + j
                        op("pe", lambda e: e.transpose(ps.t[:, j * 128:(j + 1) * 128], xT[:, kc, tt * 128:(tt + 1) * 128], identF),
                           [xT_r[kc], cst.res], [ps.res])
                    op("act", lambda e: e.activation(out=og.t[:, half * 512:(half + 1) * 512], in_=ps.t[:, :], func=AF.Copy), [ps.res], [og.res])
                S.dma("act", out_d[s, tok0 + tt * 128: tok0 + (tt + 1) * 128, :], og.t[:, :], [og.res], [])

        for s in range(NS):
            for blk in range(NBLK):
                tok0 = blk * TB
                load_x(s, tok0)
                rope_tables(s, tok0)
                if s == 0 and blk == 0:
                    for _ in range(min(n_cast, 40)):
                        prepass_one()
                    w_load_more()
                for l in range(NL):
                    if l % 2 == 0:
                        even_layer(l, blk)
                    else:
                        odd_layer(l, blk)
                final_out(s, tok0)
        S.finish()
        print(f"[build] instructions={S.n_ins} waits={S.n_wait} dmas={S.dma_i} per-engine=" + str({n: e.cnt for n, e in S.engs.items()}), flush=True)
    return nc


def _chunk(cols):
    return np.ascontiguousarray(cols.reshape(8, 128, 128).transpose(1, 0, 2)).reshape(128, 1024)


def prep_weights(w_in_even, w_out_even, w_in_odd, w_out_odd):
    wall = np.zeros((NCH, 128, 1024), np.float32)
    z64 = np.zeros((1024, 56), np.float32)
    for i in range(2):
        We = np.asarray(w_in_even[i], np.float32)
        q, k, v = We[:, 0:512], We[:, 512:576], We[:, 576:640]
        qi, ki, wi = We[:, 640:1152], We[:, 1152:1216], We[:, 1216:1224]
        ga, lin, gg, gb = We[:, 1224:1736], We[:, 1736:2248], We[:, 2248:2760], We[:, 2760:3272]
        ch = []
        for c in range(4):
            ch.append(lin[:, c * 128:(c + 1) * 128])
            ch.append(gg[:, c * 128:(c + 1) * 128])
        for c in range(4):
            ch.append(gb[:, c * 128:(c + 1) * 128])
        for c in range(4):
            ch.append(q[:, c * 128:(c + 1) * 128])
        ch.append(np.concatenate([k, k], axis=1))
        ch.append(np.concatenate([v, wi, z64], axis=1))
        for c in range(4):
            ch.append(qi[:, c * 128:(c + 1) * 128])
        ch.append(np.concatenate([ki, ki], axis=1))
        for c in range(4):
            ch.append(ga[:, c * 128:(c + 1) * 128])
        Wo = np.asarray(w_out_even[i], np.float32)
        for m in range(8):
            ch.append(Wo[:, m * 128:(m + 1) * 128])
        assert len(ch) == NCH_E
        for j, cm in enumerate(ch):
            wall[LBASE[2 * i] + j] = _chunk(cm)
        Wi = np.asarray(w_in_odd[i], np.float32)
        bg, cg, xi, gt = Wi[:, 0:1024], Wi[:, 1024:2048], Wi[:, 2048:3072], Wi[:, 3072:4096]
        ch = []
        for m in range(8):
            sl = slice(m * 128, (m + 1) * 128)
            ch += [cg[:, sl], xi[:, sl], gt[:, sl], bg[:, sl]]
        Wo = np.asarray(w_out_odd[i], np.float32)
        for m in range(8):
            ch.append(Wo[:, m * 128:(m + 1) * 128])
        assert len(ch) == NCH_O
        for j, cm in enumerate(ch):
            wall[LBASE[2 * i + 1] + j] = _chunk(cm)
    return wall


def prep_params(norm_g, final_g, conv_b_w, conv_b_bias, conv_ln_g, conv_ln_b, conv_c_w):
    par = np.zeros((128, NPAR), np.float32)
    par[:, P_G0:P_G0 + 32] = np.asarray(norm_g, np.float32).reshape(4, 8, 128).transpose(2, 0, 1).reshape(128, 32)
    par[:, P_GF:P_GF + 8] = np.asarray(final_g, np.float32).reshape(8, 128).T
    par[:, P_CBW:P_CBW + 248] = np.asarray(conv_b_w, np.float32).reshape(2, 31, 4, 128).transpose(3, 0, 2, 1).reshape(128, 248)
    par[:, P_CBB:P_CBB + 8] = np.asarray(conv_b_bias, np.float32).reshape(2, 4, 128).transpose(2, 0, 1).reshape(128, 8)
    par[:, P_LNG:P_LNG + 8] = np.asarray(conv_ln_g, np.float32).reshape(2, 4, 128).transpose(2, 0, 1).reshape(128, 8)
    par[:, P_LNB:P_LNB + 8] = np.asarray(conv_ln_b, np.float32).reshape(2, 4, 128).transpose(2, 0, 1).reshape(128, 8)
    par[:, P_CCW:P_CCW + 48] = np.asarray(conv_c_w, np.float32).reshape(2, 3, 8, 128).transpose(3, 0, 2, 1).reshape(128, 48)
    half = 32
    inv = (np.float32(10000.0) ** (-(np.arange(half, dtype=np.float32)) / np.float32(half))).astype(np.float32)
    par[:, P_INV] = inv[np.arange(128) % 32]
    par[:, P_EPS] = 1e-6
    par[:, P_CK:P_CK + NIT] = (0.5 ** (np.arange(NIT, dtype=np.float64) + 1)).astype(np.float32)[None, :]
    return par


def prep_consts():
    cst = np.zeros((128, 384), np.float32)
    cst[:, 0:128] = np.eye(128, dtype=np.float32)
    Rm = np.zeros((128, 128), np.float32)
    for hb in (0, 64):
        for d2 in range(64):
            if d2 < 32:
                Rm[hb + d2 + 32, hb + d2] = -1.0
            else:
                Rm[hb + d2 - 32, hb + d2] = 1.0
    cst[:, 128:256] = Rm
    q = np.arange(128)[:, None]
    s = np.arange(128)[None, :]
    cst[:, 256:384] = np.where(s <= q, 0.0, -1e30).astype(np.float32)
    return cst


_CACHE = {}
N_LAUNCH = 1


def run_cores(x, positions, wall, par, cst, n_cores, NS, NL):
    key = (NS, NL)
    if key not in _CACHE:
        _CACHE[key] = build_program(NS, NL)
    nc = _CACHE[key]
    in_maps = []
    for c in range(n_cores):
        in_maps.append({"x": np.ascontiguousarray(x[c * NS:(c + 1) * NS]),
                        "pos": np.ascontiguousarray(positions[c * NS:(c + 1) * NS]).astype(np.int32),
                        "wall": wall, "par": par, "cst": cst})
    res = run_bass_kernel_spmd(nc, in_maps, core_ids=list(range(n_cores)))
    return np.concatenate([r["out"] for r in res.results], axis=0)


def kernel(x, positions, norm_g, w_in_even, w_out_even, conv_b_w, conv_b_bias,
           conv_ln_g, conv_ln_b, w_in_odd, conv_c_w, w_out_odd, final_g):
    x = np.asarray(x, np.float32)
    positions = np.asarray(positions)
    wall = prep_weights(w_in_even, w_out_even, w_in_odd, w_out_odd)
    par = prep_params(norm_g, final_g, conv_b_w, conv_b_bias, conv_ln_g, conv_ln_b, conv_c_w)
    cst = prep_consts()
    n_cores = 8
    n_launch = N_LAUNCH
    NS = x.shape[0] // (n_cores * n_launch)
    outs = []
    per = n_cores * NS
    for i in range(n_launch):
        outs.append(run_cores(x[i * per:(i + 1) * per], positions[i * per:(i + 1) * per], wall, par, cst, n_cores, NS, 4))
    return np.concatenate(outs, axis=0).astype(np.float32)
```
